# Optimizing a Trainium2 kernel written in Bass

```python
import jax
import jax.numpy as jnp
from jax import lax
import numpy as np

D_MODEL = 2048
BATCH = 4
SEQ = 8192
DEPTH = 1

PLE_DIM = 256
EPS = 1e-6

ML_HEADS = 8
ML_DQK = 128
ML_DV = 256
ML_CHUNK = 64
GATE_CAP = 15.0
FGATE_BIAS = 3.0

SW_HEADS = 32
SW_KV_HEADS = 4
SW_GROUP = SW_HEADS // SW_KV_HEADS
SW_HD = 64
WINDOW = 128
SW_BLOCK = WINDOW

N_EXPERTS = 32
TOP_K = 4
D_FF = D_MODEL
SWIGLU_LIMIT = 7.0
SWIGLU_ALPHA = 1.702
MOE_BLOCK = 128

ML_QK_W = ML_HEADS * ML_DQK
ML_V_W = ML_HEADS * ML_DV
SW_Q_W = SW_HEADS * SW_HD
SW_KV_W = SW_KV_HEADS * SW_HD
IN_SIZES = (ML_QK_W, ML_QK_W, ML_V_W, ML_V_W, ML_HEADS, ML_HEADS, SW_Q_W, SW_KV_W, SW_KV_W, D_MODEL, D_MODEL)
D_IN = sum(IN_SIZES)

kernel_name = 'hybrid_mlstm_swa_moe_block'


def rms_norm(x, w):
    xf = x.astype(jnp.float32)
    y = xf * lax.rsqrt(jnp.mean(xf * xf, axis=-1, keepdims=True) + EPS)
    return (y * w.astype(jnp.float32)).astype(x.dtype)


def alibi_slopes(n_heads):
    return 2.0 ** (-8.0 * jnp.arange(1, n_heads + 1, dtype=jnp.float32) / n_heads)


def mlstm_branch(q, k, v, o_pre, i_pre, f_pre, norm_w):
    f32 = jnp.float32
    B, S = q.shape[0], q.shape[1]
    nc = S // ML_CHUNK

    def chunks(a, d):
        return a.astype(f32).reshape(B, nc, ML_CHUNK, ML_HEADS, d).transpose(1, 0, 3, 2, 4)

    def gate_chunks(a):
        return a.reshape(B, nc, ML_CHUNK, ML_HEADS).transpose(1, 0, 3, 2)

    qc = chunks(q, ML_DQK) * (ML_DQK ** -0.5)
    kc = chunks(k, ML_DQK)
    vc = chunks(v, ML_DV)
    log_i = gate_chunks(GATE_CAP * jnp.tanh(i_pre.astype(f32) / GATE_CAP))
    log_f = gate_chunks(jax.nn.log_sigmoid(GATE_CAP * jnp.tanh(f_pre.astype(f32) / GATE_CAP)))
    causal = jnp.tril(jnp.ones((ML_CHUNK, ML_CHUNK), dtype=bool))

    def step(carry, xs):
        C, n, m = carry
        qb, kb, vb, li, lf = xs
        b = jnp.cumsum(lf, axis=-1)
        g = b[..., -1]
        log_d = jnp.where(causal, b[..., :, None] - b[..., None, :] + li[..., None, :], -jnp.inf)
        log_inter = b + m[..., None]
        m_row = jnp.maximum(log_inter, log_d.max(axis=-1))
        inter = jnp.exp(log_inter - m_row)
        s = jnp.einsum('bhid,bhjd->bhij', qb, kb) * jnp.exp(log_d - m_row[..., None])
        num = jnp.einsum('bhij,bhjv->bhiv', s, vb) + inter[..., None] * jnp.einsum('bhid,bhdv->bhiv', qb, C)
        den = s.sum(axis=-1) + inter * jnp.einsum('bhid,bhd->bhi', qb, n)
        h = num / jnp.maximum(jnp.abs(den), jnp.exp(-m_row))[..., None]
        log_w = g[..., None] - b + li
        m_new = jnp.maximum(g + m, log_w.max(axis=-1))
        w = jnp.exp(log_w - m_new[..., None])
        decay = jnp.exp(g + m - m_new)
        C_new = decay[..., None, None] * C + jnp.einsum('bhj,bhjd,bhjv->bhdv', w, kb, vb)
        n_new = decay[..., None] * n + jnp.einsum('bhj,bhjd->bhd', w, kb)
        return (C_new, n_new, m_new), h

    init = (jnp.zeros((B, ML_HEADS, ML_DQK, ML_DV), f32),
            jnp.zeros((B, ML_HEADS, ML_DQK), f32),
            jnp.zeros((B, ML_HEADS), f32))
    _, h = lax.scan(step, init, (qc, kc, vc, log_i, log_f))
    h = h.transpose(1, 0, 3, 2, 4).reshape(B, S, ML_HEADS, ML_DV)
    h = h * lax.rsqrt(jnp.mean(h * h, axis=-1, keepdims=True) + EPS) * norm_w.astype(f32).reshape(ML_HEADS, ML_DV)
    h = h.reshape(B, S, ML_V_W) * jax.nn.sigmoid(o_pre.astype(f32))
    return h.astype(q.dtype)


def swa_branch(q, k, v, q_norm_w, k_norm_w, sinks):
    f32 = jnp.float32
    B, S = q.shape[0], q.shape[1]
    nb = S // SW_BLOCK
    q = rms_norm(q.reshape(B, S, SW_HEADS, SW_HD), q_norm_w)
    k = rms_norm(k.reshape(B, S, SW_KV_HEADS, SW_HD), k_norm_w)
    v = v.reshape(B, S, SW_KV_HEADS, SW_HD)
    qb = q.reshape(B, nb, SW_BLOCK, SW_KV_HEADS, SW_GROUP, SW_HD)

    def band(a):
        a = a.reshape(B, nb, SW_BLOCK, SW_KV_HEADS, SW_HD)
        prev = jnp.pad(a[:, :-1], ((0, 0), (1, 0), (0, 0), (0, 0), (0, 0)))
        return jnp.concatenate([prev, a], axis=2)

    kb, vb = band(k), band(v)
    scores = jnp.einsum('bnqhgd,bnkhd->bnhgqk', qb, kb).astype(f32) * (SW_HD ** -0.5)
    q_pos = jnp.arange(SW_BLOCK)[:, None] + SW_BLOCK
    k_pos = jnp.arange(2 * SW_BLOCK)[None, :]
    dist = q_pos - k_pos
    in_window = (dist >= 0) & (dist < WINDOW)
    has_prev = (jnp.arange(nb) > 0)[:, None, None] | (k_pos >= SW_BLOCK)[None]
    mask = (in_window[None] & has_prev)[None, :, None, None]
    slopes = alibi_slopes(SW_HEADS).reshape(SW_KV_HEADS, SW_GROUP)
    scores = scores - slopes[:, :, None, None] * dist.astype(f32)
    scores = jnp.where(mask, scores, -jnp.inf)
    sink = sinks.astype(f32).reshape(SW_KV_HEADS, SW_GROUP, 1, 1)
    m = jnp.maximum(scores.max(axis=-1, keepdims=True), sink)
    e = jnp.exp(scores - m)
    probs = e / (e.sum(axis=-1, keepdims=True) + jnp.exp(sink - m))
    out = jnp.einsum('bnhgqk,bnkhd->bnqhgd', probs.astype(v.dtype), vb)
    return out.reshape(B, S, SW_Q_W)


def moe_ffn(xn, w_router, b_router, w_up, b_up, w_down, b_down):
    B, S, D = xn.shape
    T = B * S
    xt = xn.reshape(T, D)
    logits = (xt @ w_router + b_router).astype(jnp.float32)
    top_val, top_idx = lax.top_k(logits, TOP_K)
    gates = jax.nn.softmax(top_val, axis=-1).astype(xn.dtype)
    flat_e = top_idx.reshape(-1)
    flat_tok = jnp.repeat(jnp.arange(T, dtype=jnp.int32), TOP_K)
    flat_g = gates.reshape(-1)
    order = jnp.argsort(flat_e)
    e_sorted, tok_sorted, g_sorted = flat_e[order], flat_tok[order], flat_g[order]
    counts = jnp.bincount(flat_e, length=N_EXPERTS)
    starts = jnp.cumsum(counts) - counts
    padded = (counts + MOE_BLOCK - 1) // MOE_BLOCK * MOE_BLOCK
    pad_ends = jnp.cumsum(padded)
    pad_starts = pad_ends - padded
    dest = pad_starts[e_sorted] + (jnp.arange(T * TOP_K) - starts[e_sorted])
    P = T * TOP_K + N_EXPERTS * MOE_BLOCK
    n_blocks = P // MOE_BLOCK
    buf_tok = jnp.zeros((P,), jnp.int32).at[dest].set(tok_sorted)
    buf_g = jnp.zeros((P,), xn.dtype).at[dest].set(g_sorted)
    block_e = jnp.minimum(jnp.searchsorted(pad_ends, jnp.arange(n_blocks) * MOE_BLOCK, side='right'), N_EXPERTS - 1)

    def expert_block(args):
        tok, g, e = args
        hb = xt[tok] @ w_up[e] + b_up[e]
        h_glu, h_lin = jnp.split(hb, 2, axis=-1)
        h_glu = jnp.minimum(h_glu, SWIGLU_LIMIT)
        h_lin = jnp.clip(h_lin, -SWIGLU_LIMIT, SWIGLU_LIMIT)
        act = h_glu * jax.nn.sigmoid(SWIGLU_ALPHA * h_glu) * (h_lin + 1.0)
        return (act @ w_down[e] + b_down[e]) * g[:, None]

    ys = lax.map(expert_block, (buf_tok.reshape(n_blocks, MOE_BLOCK), buf_g.reshape(n_blocks, MOE_BLOCK), block_e))
    out = jnp.zeros_like(xt).at[buf_tok].add(ys.reshape(P, D))
    return out.reshape(B, S, D)


def setup_inputs(seed: int = 0) -> dict:
    key = jax.random.key(seed)
    ks = jax.random.split(key, 24)
    f32 = jnp.float32
    nrm = lambda k, shape, scale: jax.random.normal(k, shape, f32) * scale
    gain = lambda k, shape: 1.0 + 0.05 * jax.random.normal(k, shape, f32)
    return {
        'x': nrm(ks[0], (BATCH, SEQ, D_MODEL), 1.0),
        'p': nrm(ks[1], (DEPTH, BATCH, SEQ, PLE_DIM), 1.0),
        'norm_mix_w': gain(ks[2], (DEPTH, D_MODEL)),
        'w_in': nrm(ks[3], (DEPTH, D_MODEL, D_IN), D_MODEL ** -0.5),
        'ml_igate_b': nrm(ks[4], (DEPTH, ML_HEADS), 0.1),
        'ml_fgate_b': FGATE_BIAS + nrm(ks[5], (DEPTH, ML_HEADS), 0.1),
        'ml_norm_w': gain(ks[6], (DEPTH, ML_V_W)),
        'sw_q_norm_w': gain(ks[7], (DEPTH, SW_HD)),
        'sw_k_norm_w': gain(ks[8], (DEPTH, SW_HD)),
        'sw_sinks': nrm(ks[9], (DEPTH, SW_HEADS), 0.5),
        'w_branch_a': nrm(ks[10], (DEPTH, ML_V_W, D_MODEL), ML_V_W ** -0.5),
        'w_branch_b': nrm(ks[11], (DEPTH, SW_Q_W, D_MODEL), SW_Q_W ** -0.5),
        'w_out': nrm(ks[12], (DEPTH, D_MODEL, D_MODEL), D_MODEL ** -0.5),
        'norm_ffn_w': gain(ks[13], (DEPTH, D_MODEL)),
        'w_router': nrm(ks[14], (DEPTH, D_MODEL, N_EXPERTS), D_MODEL ** -0.5),
        'b_router': nrm(ks[15], (DEPTH, N_EXPERTS), 0.01),
        'w_expert_up': nrm(ks[16], (DEPTH, N_EXPERTS, D_MODEL, 2 * D_FF), D_MODEL ** -0.5),
        'b_expert_up': nrm(ks[17], (DEPTH, N_EXPERTS, 2 * D_FF), 0.02),
        'w_expert_down': nrm(ks[18], (DEPTH, N_EXPERTS, D_FF, D_MODEL), D_FF ** -0.5),
        'b_expert_down': nrm(ks[19], (DEPTH, N_EXPERTS, D_MODEL), 0.02),
        'norm_ple_w': gain(ks[20], (DEPTH, D_MODEL)),
        'w_ple_gate': nrm(ks[21], (DEPTH, D_MODEL, D_MODEL), D_MODEL ** -0.5),
        'w_ple_proj': nrm(ks[22], (DEPTH, PLE_DIM, D_MODEL), PLE_DIM ** -0.5),
    }


def reference(x, p, norm_mix_w, w_in, ml_igate_b, ml_fgate_b, ml_norm_w, sw_q_norm_w, sw_k_norm_w, sw_sinks,
              w_branch_a, w_branch_b, w_out, norm_ffn_w, w_router, b_router, w_expert_up, b_expert_up,
              w_expert_down, b_expert_down, norm_ple_w, w_ple_gate, w_ple_proj):
    split_points = np.cumsum(IN_SIZES)[:-1].tolist()
    h = x
    for i in range(DEPTH):
        xn = rms_norm(h, norm_mix_w[i])
        proj = xn @ w_in[i]
        (ml_q, ml_k, ml_v, ml_o, ml_i, ml_f, sw_q, sw_k, sw_v, g_a, g_b) = jnp.split(proj, split_points, axis=-1)
        y_a = mlstm_branch(ml_q, ml_k, ml_v, ml_o, ml_i + ml_igate_b[i], ml_f + ml_fgate_b[i], ml_norm_w[i]) @ w_branch_a[i]
        y_b = swa_branch(sw_q, sw_k, sw_v, sw_q_norm_w[i], sw_k_norm_w[i], sw_sinks[i]) @ w_branch_b[i]
        mixed = jax.nn.sigmoid(g_a) * y_a + jax.nn.sigmoid(g_b) * y_b
        h = h + mixed @ w_out[i]
        h = h + moe_ffn(rms_norm(h, norm_ffn_w[i]), w_router[i], b_router[i], w_expert_up[i], b_expert_up[i],
                        w_expert_down[i], b_expert_down[i])
        gate = jax.nn.sigmoid(rms_norm(h, norm_ple_w[i]) @ w_ple_gate[i])
        h = h + gate * (p[i] @ w_ple_proj[i])
    return h
```

```python
import numpy as np
import ml_dtypes
from contextlib import ExitStack
import concourse.bass as bass
import concourse.mybir as mybir
from concourse.bass_utils import run_bass_kernel_spmd

F32 = mybir.dt.float32
BF16 = mybir.dt.bfloat16
I32 = mybir.dt.int32
U8 = mybir.dt.uint8
AF = mybir.ActivationFunctionType
ALU = mybir.AluOpType
AX = mybir.AxisListType

ENGS = ("pe", "act", "dve", "pool", "sp")
NDMASEM = 24

D = 2048
TOK = 4096
NPRE = 4096
NT_ALL = NPRE + TOK
NE = 32
CAP = 704
ETILES = [(r, min(128, CAP - r)) for r in range(0, CAP, 128)]
NSLOT = NE * CAP
TRASH = NSLOT
EPS = 1e-6
ARENA_BYTES = 200 * 1024


class Tok:
    __slots__ = ("w", "r")

    def __init__(self):
        self.w = None
        self.r = []


class Prog:
    def __init__(self):
        self.ops, self.deps, self.eng_of, self.isdma = [], [], [], []
        self.since_barrier = []

    def op(self, eng, fn, reads=(), writes=(), dma=False):
        i = len(self.ops)
        d = set()
        for t in reads:
            if t.w is not None:
                d.add(t.w)
        for t in writes:
            if t.w is not None:
                d.add(t.w)
            d.update(t.r)
        for t in reads:
            t.r.append(i)
        for t in writes:
            t.w = i
            t.r = []
        self.ops.append(fn)
        self.deps.append(d)
        self.eng_of.append(eng)
        self.isdma.append(dma)
        if dma:
            self.since_barrier.append(i)
        return i

    def dma(self, out, in_, reads=(), writes=(), eng="sp", **kw):
        return self.op(eng, lambda e: e.dma_start(out=out, in_=in_, **kw), reads, writes, dma=True)

    def barrier(self):
        last = {}
        for i, e in enumerate(self.eng_of):
            last[e] = i
        i0 = self.op("sp", lambda e: e.nop())
        self.deps[i0].update(last.values())
        self.deps[i0].update(self.since_barrier)
        self.since_barrier = []
        for e in ENGS:
            if e == "sp":
                continue
            j = self.op(e, lambda en: en.nop())
            self.deps[j].add(i0)

    def emit(self, sems, dma_sems):
        n = len(self.ops)
        has_dep = [False] * n
        for i in range(n):
            ei = self.eng_of[i]
            for d in self.deps[i]:
                if self.eng_of[d] == "pe" and ei == "pe" and not self.isdma[d]:
                    continue
                has_dep[d] = True
        sig = [None] * n
        cnt = {e: 0 for e in ENGS}
        dcnt = {e: 0 for e in ENGS}
        dma_prev = {}
        ring_guard = [None] * n
        for i in range(n):
            e = self.eng_of[i]
            if self.isdma[i]:
                k = dcnt[e]
                dcnt[e] += 1
                slot, rnd = k % NDMASEM, k // NDMASEM
                sig[i] = (dma_sems[e][slot], 16 * (rnd + 1))
                if (e, slot) in dma_prev:
                    ring_guard[i] = dma_prev[(e, slot)]
                dma_prev[(e, slot)] = i
            elif has_dep[i]:
                cnt[e] += 1
                sig[i] = (sems[e], cnt[e])
        per_eng = {e: [] for e in ENGS}
        for i in range(n):
            per_eng[self.eng_of[i]].append(i)

        def run(e, engobj):
            waited = {}
            for i in per_eng[e]:
                need = {}
                dl = list(self.deps[i])
                if ring_guard[i] is not None:
                    dl.append(ring_guard[i])
                for d in dl:
                    if self.eng_of[d] == "pe" and e == "pe" and not self.isdma[d]:
                        continue
                    s, v = sig[d]
                    key = id(s)
                    if waited.get(key, 0) >= v:
                        continue
                    if key not in need or need[key][1] < v:
                        need[key] = (s, v)
                for key, (s, v) in need.items():
                    engobj.wait_ge(s, v)
                    waited[key] = v
                ins = self.ops[i](engobj)
                if sig[i] is not None:
                    ins.then_inc(sig[i][0], 16 if self.isdma[i] else 1)

        return run


class Arena:
    def __init__(self, ar):
        self.ar = ar
        self.base = 0
        self.off = 0

    def alloc(self, shape, dt, persist=False):
        esz = 4 if dt in (F32, I32) else 2
        n = int(np.prod(shape[1:])) * esz
        n = (n + 63) // 64 * 64
        a = self.off
        self.off += n
        assert self.off <= ARENA_BYTES, f"SBUF arena overflow {self.off}"
        if persist:
            self.base = self.off
        ap = self.ar[0:shape[0], a:a + int(np.prod(shape[1:])) * esz].bitcast(dt)
        if len(shape) > 2:
            names = "abcdefg"[:len(shape) - 1]
            kw = {names[i]: shape[i + 1] for i in range(1, len(names))}
            ap = ap.rearrange("p (" + " ".join(names) + ") -> p " + " ".join(names), **kw)
        return ap

    def tile(self, shape, dt, persist=False):
        return (self.alloc(shape, dt, persist), Tok())

    def ring(self, n, shape, dt):
        return Ring([self.tile(shape, dt) for _ in range(n)])

    def reset(self):
        self.off = self.base


class Ring:
    def __init__(self, items):
        self.items = items
        self.i = 0

    def next(self):
        it = self.items[self.i % len(self.items)]
        self.i += 1
        return it


def build_nc(dbg=False):
    nc = bass.Bass("TRN2", target_bir_lowering=False)

    def din(name, shape, dt=F32):
        return nc.dram_tensor(name, list(shape), dt, kind="ExternalInput").ap()

    def dscr(name, shape, dt):
        return nc.dram_tensor(name, list(shape), dt, kind="Internal").ap()

    xall = din("xall", [NT_ALL, D])
    p_in = din("p_own", [TOK, 256])
    flag_in = din("flag", [128, 1])
    cst_f = din("cst_f", [128, 3, 128])
    cst_b = din("cst_b", [128, 4, 128], BF16)
    ebm_in = din("ebm", [128, 4 * 2 * 2 * 512], BF16)
    w_in = din("w_in", [D, 12816])
    norm_mix_w = din("norm_mix_w", [D])
    ml_ib = din("ml_igate_b", [8])
    ml_fb = din("ml_fgate_b", [8])
    ml_nw = din("ml_norm_w", [D])
    sw_qn = din("sw_q_norm_w", [64])
    sw_kn = din("sw_k_norm_w", [64])
    sw_sinks = din("sw_sinks", [32])
    w_a = din("w_branch_a", [D, D])
    w_b = din("w_branch_b", [D, D])
    w_out = din("w_out", [D, D])
    norm_ffn_w = din("norm_ffn_w", [D])
    w_router = din("w_router", [D, NE])
    b_router = din("b_router", [NE])
    w_up = din("w_expert_up", [NE, D, 2 * D])
    b_up = din("b_expert_up", [NE, 2 * D])
    w_dn = din("w_expert_down", [NE, D, D])
    b_dn = din("b_expert_down", [NE, D])
    norm_ple_w = din("norm_ple_w", [D])
    w_pg = din("w_ple_gate", [D, D])
    w_pp = din("w_ple_proj", [256, D])
    y_out = nc.dram_tensor("y", [TOK, D], F32, kind="ExternalOutput").ap()

    WS = {}
    for name, ns in (("qk", 4), ("kv", 6), ("o", 4), ("sq", 4), ("sk", 2), ("ga", 4), ("gb", 4),
                     ("a", 4), ("b", 4), ("out", 4), ("pg", 4)):
        WS[name] = dscr("ws_" + name, [ns, 128, 16, 512], BF16)
    WS_sv = dscr("ws_sv", [128, 16, 256], BF16)
    WS_g = dscr("ws_g", [128, 16, 16], BF16)
    MQT = dscr("mqt", [8, 128, TOK], BF16)
    MKT = dscr("mkt", [8, 128, TOK], BF16)
    MK = dscr("mk", [NT_ALL, 1024], BF16)
    MV = dscr("mv", [NT_ALL, 2048], BF16)
    MO = dscr("mo", [TOK, 2048], BF16)
    SQ = dscr("sq", [16, 128, TOK], BF16)
    SK = dscr("sk", [8, 128, TOK + 512], BF16)
    SV = dscr("sv", [TOK + 512, 256], BF16)
    TGA = dscr("tga", [16, 128, TOK], BF16)
    TGB = dscr("tgb", [16, 128, TOK], BF16)
    MLT = dscr("mlt", [16, 128, TOK], BF16)
    SWT = dscr("swt", [16, 128, TOK], BF16)
    MIXT = dscr("mixt", [16, 128, TOK], BF16)
    H1 = dscr("h1", [TOK, D], F32)
    XG = dscr("xg", [NSLOT + 128, D], BF16)
    YS = dscr("ys", [NSLOT + 128, D], F32)
    dbgs = {}
    if dbg:
        dbgs["h1"] = nc.dram_tensor("dbg_h1", [TOK, D], F32, kind="ExternalOutput").ap()
        dbgs["mlt"] = nc.dram_tensor("dbg_mlt", [16, 128, TOK], BF16, kind="ExternalOutput").ap()
        dbgs["swt"] = nc.dram_tensor("dbg_swt", [16, 128, TOK], BF16, kind="ExternalOutput").ap()
        dbgs["h2"] = nc.dram_tensor("dbg_h2", [TOK, D], F32, kind="ExternalOutput").ap()
        for nm, src in (("mk", MK), ("mv", MV), ("mo", MO), ("mqt", MQT), ("mkt", MKT), ("sq", SQ), ("sk", SK), ("sv", SV),
                        ("tga", TGA), ("mixt", MIXT)):
            dbgs[nm] = (nc.dram_tensor("dbg_" + nm, list(src.shape), BF16, kind="ExternalOutput").ap(), src)
        dbgs["gp"] = nc.dram_tensor("dbg_gp", [128, NT_ALL // 128 * 16], F32, kind="ExternalOutput").ap()

    P = Prog()
    with ExitStack() as es:
        arena_t = es.enter_context(nc.sbuf_tensor("arena", [128, ARENA_BYTES], U8))
        ps_t = es.enter_context(nc.psum_tensor("ps", [128, 8, 512], F32))
        sems = {e: es.enter_context(nc.semaphore("s_" + e)) for e in ENGS}
        dsems = {e: [es.enter_context(nc.semaphore(f"d_{e}{i}")) for i in range(NDMASEM)] for e in ("sp", "pool", "act")}
        block = es.enter_context(nc.Block())
        A = Arena(arena_t)
        PS = [(ps_t[:, b, :], Tok()) for b in range(8)]

        def psb(b, dt=F32):
            ap, t = PS[b]
            return (ap if dt == F32 else ap.bitcast(dt)), t

        T = lambda: Tok()

        def mm(out, lhsT, rhs, start, stop, reads, writes):
            P.op("pe", lambda e: e.matmul(out, lhsT, rhs, start=start, stop=stop), reads, writes)

        def tr(out, in_, ident, reads, writes):
            P.op("pe", lambda e: e.transpose(out, in_, ident), reads, writes)

        def act(out, in_, func, reads, writes, scale=1.0, bias=None, accum=None):
            kw = {}
            if bias is not None:
                kw["bias"] = bias
            if accum is not None:
                kw["accum_out"] = accum
            P.op("act", lambda e: e.activation(out=out, in_=in_, func=func, scale=scale, **kw), reads, writes)

        def ts(eng, out, in0, s1, s2, op0, op1, reads, writes):
            if op1 is None:
                P.op(eng, lambda e: e.tensor_scalar(out=out, in0=in0, scalar1=s1, scalar2=None, op0=op0), reads, writes)
            else:
                P.op(eng, lambda e: e.tensor_scalar(out=out, in0=in0, scalar1=s1, scalar2=s2, op0=op0, op1=op1), reads, writes)

        def tt(eng, out, in0, in1, op, reads, writes):
            P.op(eng, lambda e: e.tensor_tensor(out=out, in0=in0, in1=in1, op=op), reads, writes)

        def stt(out, in0, scalar, in1, op0, op1, reads, writes):
            P.op("dve", lambda e: e.scalar_tensor_tensor(out=out, in0=in0, scalar=scalar, in1=in1, op0=op0, op1=op1), reads, writes)

        def cp(eng, out, in_, reads, writes):
            if eng == "act":
                P.op("act", lambda e: e.copy(out=out, in_=in_), reads, writes)
            else:
                P.op(eng, lambda e: e.tensor_copy(out=out, in_=in_), reads, writes)

        def memset(eng, ap, val, writes):
            P.op(eng, lambda e: e.memset(ap, val), (), writes)

        cf, cf_t = A.tile([128, 3, 128], F32, persist=True)
        cb, cb_t = A.tile([128, 4, 128], BF16, persist=True)
        P.dma(cf, cst_f, writes=[cf_t])
        P.dma(cb, cst_b, writes=[cb_t])
        IDF, TRI, ONESF = cf[:, 0, :], cf[:, 1, :], cf[:, 2, :]
        IDB, NEGM, BD64, TRIS = cb[:, 0, :], cb[:, 1, :], cb[:, 2, :], cb[:, 3, :]
        neghalf, neghalf_t = A.tile([128, 512], F32, persist=True)
        memset("pool", neghalf, -0.5, [neghalf_t])
        flag, flag_t = A.tile([128, 1], F32, persist=True)
        P.dma(flag, flag_in, writes=[flag_t])
        NTILES = NT_ALL // 128
        GP, GP_t = A.tile([128, NTILES, 16], F32, persist=True)
        SLOT, SLOT_t = A.tile([128, 32, 4], I32, persist=True)
        GATE, GATE_t = A.tile([128, 32, 4], F32, persist=True)
        rs, rs_t = A.tile([128, 4, 16], F32, persist=True)
        for i, v in enumerate((norm_mix_w, norm_ffn_w, norm_ple_w)):
            P.dma(rs[:, i, :], v.rearrange("(k p) -> p k", p=128), writes=[rs_t], allow_slow_non_contiguous=True)
        half_t = T()
        junk, junk_t = A.tile([128, D], BF16, persist=True)
        rsh, _ = A.tile([128, 1], F32, persist=True)
        memset("pool", rsh, 0.5, [half_t])

        epsb, epsb_t = A.tile([128, 1], F32, persist=True)
        memset("pool", epsb, EPS, [epsb_t])

        def rsqrt_act(out, in_, reads, writes, scale=1.0, eps=False):
            if eps:
                act(out, in_, AF.Ln, list(reads) + [epsb_t], writes, scale=scale, bias=epsb[:, 0:1])
            else:
                act(out, in_, AF.Ln, reads, writes, scale=scale)
            act(out, out, AF.Exp, writes, writes, scale=-0.5)

        cast_i = [0]

        def cast_scale(out, in_, scale_ap, reads, writes, engs=("act", "dve", "act", "dve", "act", "dve", "act")):
            e = engs[cast_i[0] % len(engs)]
            cast_i[0] += 1
            if e == "act":
                if scale_ap is None:
                    P.op("act", lambda en: en.copy(out=out, in_=in_), reads, writes)
                else:
                    P.op("act", lambda en: en.activation(out=out, in_=in_, func=AF.Copy, scale=scale_ap), reads, writes)
            else:
                if scale_ap is None:
                    P.op(e, lambda en: en.tensor_copy(out=out, in_=in_), reads, writes)
                else:
                    P.op(e, lambda en: en.tensor_scalar(out=out, in0=in_, scalar1=scale_ap, scalar2=None, op0=ALU.mult), reads, writes)

        ws_tok = {k: T() for k in list(WS) + ["sv", "g"]}
        stg = A.ring(2, [128, 2048], F32)
        cst = A.ring(2, [128, 2048], BF16)
        kpad, kpad_t = A.tile([128, 1024], BF16)
        memset("pool", kpad, 0.0, [kpad_t])

        def cast_matrix(src, col0, ncols, dst, dst_tok, scale_idx, fscale=None, kchunks=16):
            for c0 in range(0, ncols, 2048):
                nc_ = min(2048, ncols - c0)
                for kc in range(kchunks):
                    s_ap, s_t = stg.next()
                    P.dma(s_ap[:, 0:nc_], src[kc * 128:(kc + 1) * 128, col0 + c0:col0 + c0 + nc_], writes=[s_t])
                    c_ap, c_t = cst.next()
                    if scale_idx is not None:
                        sc = rs[:, scale_idx, kc:kc + 1]
                        rd = [s_t, rs_t]
                    elif fscale is not None:
                        sc, rd = fscale, [s_t, half_t]
                    else:
                        sc, rd = None, [s_t]
                    cast_scale(c_ap[:, 0:nc_], s_ap[:, 0:nc_], sc, rd, [c_t], engs=("act", "dve"))
                    s0 = c0 // 512
                    ns = nc_ // 512
                    P.dma(dst[s0:s0 + ns, :, kc, :].rearrange("s p c -> p s c"),
                          c_ap[:, 0:nc_].rearrange("p (s c) -> p s c", c=512), reads=[c_t], writes=[dst_tok], eng="pool")

        C_Q, C_K, C_V, C_O, C_G, C_SQ, C_SK, C_SV, C_GA, C_GB = 0, 1024, 2048, 4096, 6144, 6160, 8208, 8464, 8720, 10768
        cast_matrix(w_in, C_Q, 2048, WS["qk"], ws_tok["qk"], 0)
        cast_matrix(w_in, C_K, 3072, WS["kv"], ws_tok["kv"], 0)
        cast_matrix(w_in, C_O, 2048, WS["o"], ws_tok["o"], 0)
        cast_matrix(w_in, C_SQ, 2048, WS["sq"], ws_tok["sq"], 0)
        cast_matrix(w_in, C_GA, 2048, WS["ga"], ws_tok["ga"], 0)
        cast_matrix(w_in, C_GB, 2048, WS["gb"], ws_tok["gb"], 0)
        cast_matrix(w_a, 0, 2048, WS["a"], ws_tok["a"], None)
        cast_matrix(w_b, 0, 2048, WS["b"], ws_tok["b"], None)
        cast_matrix(w_out, 0, 2048, WS["out"], ws_tok["out"], None, fscale=rsh[:, 0:1])
        cast_matrix(w_pg, 0, 2048, WS["pg"], ws_tok["pg"], 2)
        for kc in range(16):
            s_ap, s_t = stg.next()
            P.dma(s_ap[:, 0:16], w_in[kc * 128:(kc + 1) * 128, C_G:C_G + 16], writes=[s_t])
            P.dma(s_ap[:, 512:1024], w_in[kc * 128:(kc + 1) * 128, C_SK:C_SK + 512], writes=[s_t])
            c_ap, c_t = cst.next()
            sc = rs[:, 0, kc:kc + 1]
            ts("dve", c_ap[:, 0:16], s_ap[:, 0:16], sc, None, ALU.mult, None, [s_t, rs_t], [c_t])
            ts("dve", c_ap[:, 512:1024], s_ap[:, 512:1024], sc, None, ALU.mult, None, [s_t, rs_t], [c_t])
            P.dma(WS_g[:, kc, :], c_ap[:, 0:16], reads=[c_t], writes=[ws_tok["g"]])
            P.dma(WS_sv[:, kc, :], c_ap[:, 768:1024], reads=[c_t], writes=[ws_tok["sv"]])
            kp4 = kpad.rearrange("p (g ab c) -> p g ab c", ab=2, c=128)
            ksrc = c_ap[:, 512:768].rearrange("p (g c) -> p g c", c=64)
            cp("pool", kp4[:, :, 0, 0:64], ksrc, [c_t], [kpad_t])
            cp("pool", kp4[:, :, 1, 64:128], ksrc, [c_t], [kpad_t])
            P.dma(WS["sk"][0:2, :, kc, :].rearrange("s p c -> p s c"), kpad.rearrange("p (s c) -> p s c", c=512),
                  reads=[kpad_t], writes=[ws_tok["sk"]])
        P.barrier()
        A.reset()

        xin = A.ring(2, [128, D], F32)
        xnb = A.ring(2, [128, D], BF16)
        xnT = A.ring(2, [128, 16, 512], BF16)
        slab = A.ring(3, [128, 16, 512], BF16)
        sm = A.ring(4, [128, 4], F32)
        oev = A.ring(4, [128, 512], BF16)
        sqt = A.ring(2, [128, 512], BF16)
        r0 = A.ring(2, [128, 512], F32)
        r1 = A.ring(2, [128, 512], F32)
        wq8, wq8_t = A.tile([128, 2], F32)
        for hh in range(2):
            P.dma(wq8[hh * 64:(hh + 1) * 64, 0:1], sw_qn.rearrange("(p o) -> p o", o=1), writes=[wq8_t], allow_slow_non_contiguous=True)
            P.dma(wq8[hh * 64:(hh + 1) * 64, 1:2], sw_kn.rearrange("(p o) -> p o", o=1), writes=[wq8_t], allow_slow_non_contiguous=True)
        ts("dve", wq8[:, 0:1], wq8[:, 0:1], 0.125, None, ALU.mult, None, [wq8_t], [wq8_t])
        scr_tok = {k: T() for k in ("mqt", "mkt", "mk", "mv", "mo", "sq", "sk", "sv", "tga", "tgb")}
        evi = [0]
        psr = [0]

        def next_ps(lo=0, hi=6):
            b = lo + psr[0] % (hi - lo)
            psr[0] += 1
            return psb(b)

        def rms_rstd(x_ap, x_t, ncols):
            s_ap, s_t = sm.next()
            act(junk[:, 0:ncols], x_ap, AF.Square, [x_t], [junk_t, s_t], accum=s_ap[:, 0:1])
            rsqrt_act(s_ap[:, 2:3], s_ap[:, 0:1], [s_t], [s_t], scale=1.0 / ncols, eps=True)
            return s_ap[:, 2:3], s_t

        n_pre_st = NPRE // 512
        n_st = NT_ALL // 512
        for st in range(n_st):
            own = st >= n_pre_st
            halo = (st == n_pre_st - 1)
            ost = st - n_pre_st
            xt_ap, xt_t = xnT.next()
            for t4 in range(4):
                tix = st * 4 + t4
                x_ap, x_t = xin.next()
                P.dma(x_ap, xall[tix * 128:(tix + 1) * 128, :], writes=[x_t])
                rstd, rstd_t = rms_rstd(x_ap, x_t, D)
                xb_ap, xb_t = xnb.next()
                act(xb_ap, x_ap, AF.Copy, [x_t, rstd_t], [xb_t], scale=rstd)
                for half in range(2):
                    pb, pb_t = psb(6 + half, BF16)
                    for k in range(8):
                        kc = half * 8 + k
                        tr(pb[:, k * 128:(k + 1) * 128], xb_ap[:, kc * 128:(kc + 1) * 128], IDB, [xb_t, cb_t], [pb_t])
                    cp("dve" if half else "act", xt_ap[:, half * 8:(half + 1) * 8, t4 * 128:(t4 + 1) * 128],
                       pb[:, 0:1024].rearrange("p (k c) -> p k c", c=128), [pb_t], [xt_t])

            def fm_group(wname, sidx, nblk, handler):
                w_ap, w_t = slab.next()
                P.dma(w_ap, WS[wname][sidx], reads=[ws_tok[wname]], writes=[w_t])
                for j in range(nblk):
                    p_ap, p_t = next_ps()
                    for kc in range(16):
                        mm(p_ap, w_ap[:, kc, j * 128:(j + 1) * 128], xt_ap[:, kc, :], kc == 0, kc == 15, [w_t, xt_t], [p_t])
                    handler(sidx * 4 + j, p_ap, p_t)

            def tm_group(w_ap, w_t, ncols, handler):
                for t4 in range(4):
                    p_ap, p_t = next_ps()
                    for kc in range(16):
                        mm(p_ap[:, 0:ncols], xt_ap[:, kc, t4 * 128:(t4 + 1) * 128], w_ap[:, kc, 0:ncols], kc == 0, kc == 15, [w_t, xt_t], [p_t])
                    handler(t4, p_ap, p_t)

            def ev_store(p_ap, p_t, ncols, dst, dst_tok, func=AF.Copy, scale=1.0):
                o_ap, o_t = oev.next()
                evi[0] += 1
                if func == AF.Copy and evi[0] % 2 == 0 and scale == 1.0:
                    cp("dve", o_ap[:, 0:ncols], p_ap[:, 0:ncols], [p_t], [o_t])
                else:
                    act(o_ap[:, 0:ncols], p_ap[:, 0:ncols], func, [p_t], [o_t], scale=scale)
                P.dma(dst, o_ap[:, 0:ncols], reads=[o_t], writes=[dst_tok], eng="pool")

            def qknorm(p_ap, p_t, wcol, dst, dst_tok):
                sq_ap, sq_t = sqt.next()
                act(sq_ap, p_ap, AF.Square, [p_t], [sq_t])
                q_ap, q_t = psb(6)
                mm(q_ap, BD64, sq_ap, True, True, [sq_t, cb_t], [q_t])
                b_ap, b_t = r1.next()
                rsqrt_act(b_ap, q_ap, [q_t], [b_t], eps=True)
                o_ap, o_t = oev.next()
                stt(o_ap, p_ap, wq8[:, wcol:wcol + 1], b_ap, ALU.mult, ALU.mult, [p_t, b_t, wq8_t], [o_t])
                P.dma(dst, o_ap, reads=[o_t], writes=[dst_tok], eng="pool")

            tsl = slice(ost * 512, (ost + 1) * 512)
            for s in range(6):
                w_ap, w_t = slab.next()
                P.dma(w_ap, WS["kv"][s], reads=[ws_tok["kv"]], writes=[w_t])
                if s < 2:
                    tm_group(w_ap, w_t, 512, lambda t4, p, pt, s=s: ev_store(
                        p, pt, 512, MK[(st * 4 + t4) * 128:(st * 4 + t4 + 1) * 128, s * 512:(s + 1) * 512], scr_tok["mk"]))
                else:
                    tm_group(w_ap, w_t, 512, lambda t4, p, pt, s=s: ev_store(
                        p, pt, 512, MV[(st * 4 + t4) * 128:(st * 4 + t4 + 1) * 128, (s - 2) * 512:(s - 1) * 512], scr_tok["mv"]))
            w_ap, w_t = slab.next()
            P.dma(w_ap[:, :, 0:16], WS_g, reads=[ws_tok["g"]], writes=[w_t])
            P.dma(w_ap[:, :, 256:512], WS_sv, reads=[ws_tok["sv"]], writes=[w_t])
            tm_group(w_ap, w_t, 16, lambda t4, p, pt: cp("dve", GP[:, st * 4 + t4, :], p[:, 0:16], [pt], [GP_t]))
            if own or halo:
                base = (ost + 1) * 512

                def sv_h(t4, p, pt):
                    o_ap, o_t = oev.next()
                    cp("act", o_ap[:, 0:256], p[:, 0:256], [pt], [o_t])
                    P.dma(SV[base + t4 * 128:base + (t4 + 1) * 128, :], o_ap[:, 0:256], reads=[o_t], writes=[scr_tok["sv"]], eng="pool")
                for t4 in range(4):
                    p_ap, p_t = next_ps()
                    for kc in range(16):
                        mm(p_ap[:, 0:256], xt_ap[:, kc, t4 * 128:(t4 + 1) * 128], w_ap[:, kc, 256:512], kc == 0, kc == 15, [w_t, xt_t], [p_t])
                    sv_h(t4, p_ap, p_t)
                for s in range(2):
                    fm_group("sk", s, 4, lambda blk, p, pt: qknorm(p, pt, 1, SK[blk, :, base:base + 512], scr_tok["sk"]))
            if own:
                for s in range(4):
                    w_ap, w_t = slab.next()
                    P.dma(w_ap, WS["o"][s], reads=[ws_tok["o"]], writes=[w_t])
                    tm_group(w_ap, w_t, 512, lambda t4, p, pt, s=s: ev_store(
                        p, pt, 512, MO[(ost * 4 + t4) * 128:(ost * 4 + t4 + 1) * 128, s * 512:(s + 1) * 512], scr_tok["mo"],
                        func=AF.Tanh, scale=0.5))
                for s in range(4):
                    if s < 2:
                        fm_group("qk", s, 4, lambda blk, p, pt: ev_store(p, pt, 512, MQT[blk, :, tsl], scr_tok["mqt"], scale=128 ** -0.5))
                    else:
                        fm_group("qk", s, 4, lambda blk, p, pt: ev_store(p, pt, 512, MKT[blk - 8, :, tsl], scr_tok["mkt"]))
                for s in range(4):
                    fm_group("sq", s, 4, lambda blk, p, pt: qknorm(p, pt, 0, SQ[blk, :, tsl], scr_tok["sq"]))
                for s in range(4):
                    fm_group("ga", s, 4, lambda blk, p, pt: ev_store(p, pt, 512, TGA[blk, :, tsl], scr_tok["tga"], func=AF.Tanh, scale=0.5))
                for s in range(4):
                    fm_group("gb", s, 4, lambda blk, p, pt: ev_store(p, pt, 512, TGB[blk, :, tsl], scr_tok["tgb"], func=AF.Tanh, scale=0.5))
        P.barrier()
        A.reset()
        if dbg:
            for nm in ("mk", "mv", "mo", "mqt", "mkt", "sq", "sk", "sv", "tga"):
                dst, src = dbgs[nm]
                if len(src.shape) == 3:
                    for i in range(src.shape[0]):
                        P.dma(dst[i], src[i], writes=[T()])
                else:
                    n0 = src.shape[0]
                    for i in range(0, n0, 512):
                        P.dma(dst[i:min(n0, i + 512)], src[i:min(n0, i + 512)], writes=[T()])
            P.dma(dbgs["gp"], GP.rearrange("p a b -> p (a b)"), reads=[GP_t], writes=[T()])
            P.barrier()

        gbb, gbb_t = A.tile([128, 16], F32)
        P.dma(gbb[:, 0:8], ml_ib.partition_broadcast(128), writes=[gbb_t])
        P.dma(gbb[:, 8:16], ml_fb.partition_broadcast(128), writes=[gbb_t])
        LI, LI_t = A.tile([128, NTILES, 8], F32)
        LF, LF_t = A.tile([128, NTILES, 8], F32)
        gz, gz_t = A.tile([128, NTILES, 16], F32)
        tt("dve", gz, GP, gbb.unsqueeze(1).to_broadcast([128, NTILES, 16]), ALU.add, [GP_t, gbb_t], [gz_t])
        act(gz, gz, AF.Tanh, [gz_t], [gz_t], scale=1.0 / 15.0)
        ts("dve", LI, gz[:, :, 0:8], 15.0, None, ALU.mult, None, [gz_t], [LI_t])
        act(gz[:, :, 8:16], gz[:, :, 8:16], AF.Exp, [gz_t], [gz_t], scale=-15.0)
        ts("dve", gz[:, :, 8:16], gz[:, :, 8:16], 1.0, None, ALU.add, None, [gz_t], [gz_t])
        act(gz[:, :, 8:16], gz[:, :, 8:16], AF.Ln, [gz_t], [gz_t])
        ts("dve", LF, gz[:, :, 8:16], -1.0, None, ALU.mult, None, [gz_t], [LF_t])
        nwb, nwb_t = A.tile([128, D], F32)
        P.dma(nwb, ml_nw.partition_broadcast(128), writes=[nwb_t])
        ts("dve", nwb, nwb, 0.5, None, ALU.mult, None, [nwb_t], [nwb_t])
        Cst, C_t = A.tile([128, 8, 257], F32)
        Cb, Cb_t = A.tile([128, 8, 257], BF16)
        memset("pool", Cst, 0.0, [C_t])
        memset("pool", Cb, 0.0, [Cb_t])
        vaug = A.ring(2, [128, 8, 257], BF16)
        for va, vt in vaug.items:
            memset("pool", va[:, :, 256:257], 1.0, [vt])
        ktk = A.ring(2, [128, 1024], BF16)
        qTs = A.ring(2, [128, 8, 512], BF16)
        kTs = A.ring(2, [128, 8, 512], BF16)
        mo_r = A.ring(2, [128, D], BF16)
        Gt = A.ring(2, [128, D], F32)
        lfb = A.ring(2, [128, 8, 128], F32)
        smm = A.ring(3, [128, 8, 8], F32)
        dT = A.ring(2, [128, 128], BF16)
        sTs = A.ring(2, [128, 128], BF16)
        t1r = A.ring(2, [128, 257], F32)
        NUM = A.ring(2, [128, 8, 257], F32)
        kw = A.ring(2, [128, 128], BF16)
        mlo = A.ring(2, [128, D], BF16)
        mlts = A.ring(2, [128, 16, 512], BF16)
        mlt_tok = T()
        pending_tail = [None]
        for tix in range(NTILES):
            own = tix >= NPRE // 128
            otix = tix - NPRE // 128
            k_ap, k_t = ktk.next()
            P.dma(k_ap, MK[tix * 128:(tix + 1) * 128, :], reads=[scr_tok["mk"]], writes=[k_t])
            v_ap, v_t = vaug.next()
            P.dma(v_ap[:, :, 0:256], MV[tix * 128:(tix + 1) * 128, :].rearrange("p (h c) -> p h c", c=256),
                  reads=[scr_tok["mv"]], writes=[v_t])
            if own and otix % 4 == 0:
                q_ap, q_t = qTs.next()
                kT_ap, kT_t = kTs.next()
                sl = slice(otix * 128, otix * 128 + 512)
                P.dma(q_ap, MQT[:, :, sl].rearrange("h p c -> p h c"), reads=[scr_tok["mqt"]], writes=[q_t])
                P.dma(kT_ap, MKT[:, :, sl].rearrange("h p c -> p h c"), reads=[scr_tok["mkt"]], writes=[kT_t])
                ms_ap, ms_t = mlts.next()
            bg, bg_t = psb(0)
            mm(bg[:, 0:8], TRI, LF[:, tix, :], True, True, [cf_t, LF_t], [bg_t])
            mm(bg[:, 8:16], ONESF, LF[:, tix, :], True, True, [cf_t, LF_t], [bg_t])
            s_ap, s_t = smm.next()
            tt("dve", s_ap[:, 0, :], LI[:, tix, :], bg[:, 0:8], ALU.subtract, [LI_t, bg_t], [s_t])
            tt("dve", s_ap[:, 1, :], s_ap[:, 0, :], bg[:, 8:16], ALU.add, [s_t, bg_t], [s_t])
            act(s_ap[:, 2, :], s_ap[:, 1, :], AF.Exp, [s_t], [s_t])
            act(s_ap[:, 3, :], bg[:, 0:8], AF.Exp, [bg_t], [s_t])
            act(s_ap[:, 4, :], bg[:, 8:16], AF.Exp, [bg_t], [s_t])
            c0 = (otix % 4) * 128
            if own:
                mo_ap, mo_t = mo_r.next()
                P.dma(mo_ap, MO[otix * 128:(otix + 1) * 128, :], reads=[scr_tok["mo"]], writes=[mo_t])
                g_ap, g_t = Gt.next()
                stt(g_ap, mo_ap, 1.0, nwb, ALU.add, ALU.mult, [mo_t, nwb_t], [g_t])
                lb_ap, lb_t = lfb.next()
                cp("pool", lb_ap, LF[:, tix, :].unsqueeze(2).to_broadcast([128, 8, 128]), [LF_t], [lb_t])
                n_ap, n_t = NUM.next()
                ss_ap = s_ap[:, 5, :]
            sts = {}

            def p1(h, s_ap=s_ap, s_t=s_t):
                bb, bb_t = psb(1)
                mm(bb[:, 0:128], lb_ap[:, h, :], TRI, True, False, [lb_t, cf_t], [bb_t])
                mm(bb[:, 0:128], IDB, NEGM, False, True, [cb_t], [bb_t])
                d_ap, d_t = dT.next()
                act(d_ap, bb[:, 0:128], AF.Exp, [bb_t, s_t], [d_t], bias=s_ap[:, 0, h:h + 1])
                sp_, sp_t = psb(2)
                mm(sp_[:, 0:128], kT_ap[:, h, c0:c0 + 128], q_ap[:, h, c0:c0 + 128], True, True, [kT_t, q_t], [sp_t])
                st_ap, st_t = sTs.next()
                tt("dve", st_ap, sp_[:, 0:128], d_ap, ALU.mult, [sp_t, d_t], [st_t])
                sts[h] = (st_ap, st_t)

            def p2(h, s_ap=s_ap, s_t=s_t):
                if own:
                    st_ap, st_t = sts[h]
                    pi, pi_t = psb(3)
                    mm(pi[:, 0:257], st_ap, v_ap[:, h, :], True, True, [st_t, v_t], [pi_t])
                    pe_, pe_t = psb(4)
                    mm(pe_[:, 0:257], q_ap[:, h, c0:c0 + 128], Cb[:, h, :], True, True, [q_t, Cb_t], [pe_t])
                    t1, t1_t = t1r.next()
                    act(t1, pe_[:, 0:257], AF.Copy, [pe_t, s_t], [t1_t], scale=s_ap[:, 3, h:h + 1])
                    tt("dve", n_ap[:, h, :], t1, pi[:, 0:257], ALU.add, [t1_t, pi_t], [n_t])
                    act(junk[:, 0:256], n_ap[:, h, 0:256], AF.Square, [n_t], [junk_t, s_t], accum=ss_ap[:, h:h + 1])
                kw_ap, kw_t = kw.next()
                act(kw_ap, k_ap[:, h * 128:(h + 1) * 128], AF.Copy, [k_t, s_t], [kw_t], scale=s_ap[:, 2, h:h + 1])
                pc, pc_t = psb(5)
                mm(pc[:, 0:257], kw_ap, v_ap[:, h, :], True, True, [kw_t, v_t], [pc_t])
                stt(Cst[:, h, :], Cst[:, h, :], s_ap[:, 4, h:h + 1], pc[:, 0:257], ALU.mult, ALU.add, [C_t, s_t, pc_t], [C_t])
                cp("act", Cb[:, h, :], Cst[:, h, :], [C_t], [Cb_t])

            def tail(s_ap=s_ap, s_t=s_t, n_ap=n_ap if own else None, n_t=n_t if own else None, ss_ap=ss_ap if own else None,
                     g_ap=g_ap if own else None, g_t=g_t if own else None, ms_ap=ms_ap if own else None, ms_t=ms_t if own else None,
                     c0=c0, otix=otix):
                den = n_ap[:, :, 256]
                tt("dve", s_ap[:, 7, :], den, den, ALU.mult, [n_t], [s_t])
                ts("dve", s_ap[:, 7, :], s_ap[:, 7, :], 1.0, None, ALU.max, None, [s_t], [s_t])
                rsqrt_act(s_ap[:, 6, :], s_ap[:, 7, :], [s_t], [s_t])
                tt("dve", s_ap[:, 7, :], s_ap[:, 6, :], s_ap[:, 6, :], ALU.mult, [s_t], [s_t])
                tt("dve", s_ap[:, 7, :], s_ap[:, 7, :], ss_ap, ALU.mult, [s_t], [s_t])
                ts("dve", s_ap[:, 7, :], s_ap[:, 7, :], 1.0 / 256, EPS, ALU.mult, ALU.add, [s_t], [s_t])
                rsqrt_act(s_ap[:, 1, :], s_ap[:, 7, :], [s_t], [s_t])
                tt("dve", s_ap[:, 1, :], s_ap[:, 1, :], s_ap[:, 6, :], ALU.mult, [s_t], [s_t])
                o_ap, o_t = mlo.next()
                for h in range(8):
                    stt(o_ap[:, h * 256:(h + 1) * 256], n_ap[:, h, 0:256], s_ap[:, 1, h:h + 1], g_ap[:, h * 256:(h + 1) * 256],
                        ALU.mult, ALU.mult, [n_t, s_t, g_t], [o_t])
                for half in range(2):
                    pb, pb_t = psb(6 + half, BF16)
                    for k in range(8):
                        kc = half * 8 + k
                        tr(pb[:, k * 128:(k + 1) * 128], o_ap[:, kc * 128:(kc + 1) * 128], IDB, [o_t, cb_t], [pb_t])
                    cp("act", ms_ap[:, half * 8:(half + 1) * 8, c0:c0 + 128], pb[:, 0:1024].rearrange("p (k c) -> p k c", c=128), [pb_t], [ms_t])
                if otix % 4 == 3:
                    sl = slice((otix - 3) * 128, (otix + 1) * 128)
                    P.dma(MLT[:, :, sl].rearrange("k p c -> p k c"), ms_ap, reads=[ms_t], writes=[mlt_tok], eng="pool")

            if own:
                p1(0)
            for h in range(8):
                if own and h + 1 < 8:
                    p1(h + 1)
                p2(h)
                if h == 2 and pending_tail[0] is not None:
                    pending_tail[0]()
                    pending_tail[0] = None
            if own:
                pending_tail[0] = tail
        if pending_tail[0] is not None:
            pending_tail[0]()
        P.barrier()
        A.reset()

        ebm, ebm_t = A.tile([128, 4, 2, 2, 512], BF16)
        P.dma(ebm, ebm_in.rearrange("p (g k a c) -> p g k a c", g=4, k=2, a=2), writes=[ebm_t])
        eb0, eb0_t = A.tile([128, 4, 2, 512], BF16)
        ts("dve", eb0, ebm[:, :, 0, :, :], flag[:, 0:1], None, ALU.mult, None, [ebm_t, flag_t], [eb0_t])
        sinkN, sinkE_t = A.tile([128, 32], F32)
        P.dma(sinkN, sw_sinks.partition_broadcast(128), writes=[sinkE_t])
        act(sinkN, sinkN, AF.Exp, [sinkE_t], [sinkE_t])
        sinkE = sinkN.rearrange("p (g j a) -> p g a j", g=4, a=2)
        sqs = A.ring(2, [128, 16, 512], BF16)
        sks = A.ring(2, [128, 8, 640], BF16)
        vau = A.ring(3, [128, 4, 65], BF16)
        for va, vt in vau.items:
            memset("pool", va[:, :, 64:65], 1.0, [vt])
        eT = A.ring(8, [128, 512], BF16)
        eM = A.ring(8, [128, 512], BF16)
        rden = A.ring(2, [128, 2, 4], F32)
        swo = A.ring(2, [128, 512], BF16)
        swts = A.ring(2, [128, 16, 512], BF16)
        swt_tok = T()
        vprev = None
        pending_pv = [None]
        for n in range(-1, 32):
            va, vt = vau.next()
            r0_ = 512 + n * 128
            P.dma(va[:, :, 0:64], SV[r0_:r0_ + 128, :].rearrange("p (g c) -> p g c", c=64), reads=[scr_tok["sv"]], writes=[vt])
            if n < 0:
                vprev = (va, vt)
                continue
            if n % 4 == 0:
                q_ap, q_t = sqs.next()
                P.dma(q_ap, SQ[:, :, n * 128:n * 128 + 512].rearrange("k p c -> p k c"), reads=[scr_tok["sq"]], writes=[q_t])
                k_ap, k_t = sks.next()
                P.dma(k_ap, SK[:, :, 384 + n * 128:384 + n * 128 + 640].rearrange("k p c -> p k c"), reads=[scr_tok["sk"]], writes=[k_t])
                ws_ap, ws_t = swts.next()
            c0 = (n % 4) * 128
            for g in range(4):
                em = {}
                for kb in range(2):
                    kc0 = c0 + kb * 128
                    for ab in range(2):
                        p_ap, p_t = psb(kb * 2 + ab)
                        mm(p_ap.rearrange("p (j c) -> p j c", c=128), k_ap[:, g * 2 + ab, kc0:kc0 + 128], q_ap[:, 4 * g:4 * g + 4, c0:c0 + 128], True, True, [k_t, q_t], [p_t])
                        e_ap, e_t = eT.next()
                        act(e_ap, p_ap, AF.Exp, [p_t], [e_t])
                        m_ap, m_t = eM.next()
                        if n == 0 and kb == 0:
                            msk, msk_t = eb0[:, g, ab, :], eb0_t
                        else:
                            msk, msk_t = ebm[:, g, kb, ab, :], ebm_t
                        tt("pool" if ab else "dve", m_ap, e_ap, msk, ALU.mult, [e_t, msk_t], [m_t])
                        em[(kb, ab)] = (m_ap, m_t)

                def pv_phase(em=em, g=g, n=n, c0=c0, vprev=vprev, va=va, vt=vt, ws_ap=ws_ap, ws_t=ws_t):
                    o_ap, o_t = swo.next()
                    rd_ap, rd_t = rden.next()
                    for ab in range(2):
                        po, po_t = psb(4 + ab)
                        pov = po[:, 0:260].rearrange("p (j c) -> p j c", c=65)
                        for j in range(4):
                            for kb in range(2):
                                m_ap, m_t = em[(kb, ab)]
                                vv, vvt = (vprev if kb == 0 else (va, vt))
                                mm(pov[:, j, :], m_ap[:, j * 128:(j + 1) * 128], vv[:, g, :], kb == 0, kb == 1, [m_t, vvt], [po_t])
                        tt("dve", rd_ap[:, ab, :], pov[:, :, 64], sinkE[:, g, ab, :], ALU.add, [po_t, sinkE_t], [rd_t])
                        P.op("dve", lambda e, rd_ap=rd_ap, ab=ab: e.reciprocal(out=rd_ap[:, ab, :], in_=rd_ap[:, ab, :]), [rd_t], [rd_t])
                        ov = o_ap.rearrange("p (j a c) -> p j a c", a=2, c=64)
                        tt("dve", ov[:, :, ab, :], pov[:, :, 0:64], rd_ap[:, ab, :].unsqueeze(2).to_broadcast([128, 4, 64]), ALU.mult,
                           [po_t, rd_t], [o_t])
                    pb, pb_t = psb(6 + g % 2, BF16)
                    for j in range(4):
                        tr(pb[:, j * 128:(j + 1) * 128], o_ap[:, j * 128:(j + 1) * 128], IDB, [o_t, cb_t], [pb_t])
                    cp("act", ws_ap[:, 4 * g:4 * g + 4, c0:c0 + 128], pb[:, 0:512].rearrange("p (k c) -> p k c", c=128), [pb_t], [ws_t])
                    if n % 4 == 3 and g == 3:
                        sl = slice((n - 3) * 128, (n + 1) * 128)
                        P.dma(SWT[:, :, sl].rearrange("k p c -> p k c"), ws_ap, reads=[ws_t], writes=[swt_tok], eng="pool")

                if pending_pv[0] is not None:
                    pending_pv[0]()
                pending_pv[0] = pv_phase
            vprev = (va, vt)
        if pending_pv[0] is not None:
            pending_pv[0]()
        if dbg:
            for i in range(16):
                P.dma(dbgs["mlt"][i], MLT[i], reads=[mlt_tok], writes=[T()])
                P.dma(dbgs["swt"][i], SWT[i], reads=[swt_tok], writes=[T()])
        P.barrier()
        A.reset()

        mls = A.ring(2, [128, 16, 512], BF16)
        sws = A.ring(2, [128, 16, 512], BF16)
        was = A.ring(2, [128, 16, 512], BF16)
        wbs = A.ring(2, [128, 16, 512], BF16)
        tg = A.ring(4, [128, 512], BF16)
        uv = A.ring(4, [128, 512], F32)
        mxo = A.ring(3, [128, 512], BF16)
        mixt_tok = T()
        for st in range(8):
            tsl = slice(st * 512, (st + 1) * 512)
            ml_ap, ml_t = mls.next()
            sw_ap, sw_t = sws.next()
            P.dma(ml_ap, MLT[:, :, tsl].rearrange("k p c -> p k c"), reads=[mlt_tok], writes=[ml_t])
            P.dma(sw_ap, SWT[:, :, tsl].rearrange("k p c -> p k c"), reads=[swt_tok], writes=[sw_t])
            for s in range(4):
                wa_ap, wa_t = was.next()
                wb_ap, wb_t = wbs.next()
                P.dma(wa_ap, WS["a"][s], reads=[ws_tok["a"]], writes=[wa_t])
                P.dma(wb_ap, WS["b"][s], reads=[ws_tok["b"]], writes=[wb_t])
                for j in range(4):
                    ch = 4 * s + j
                    ta_ap, ta_t = tg.next()
                    tb_ap, tb_t = tg.next()
                    P.dma(ta_ap, TGA[ch, :, tsl], reads=[scr_tok["tga"]], writes=[ta_t])
                    P.dma(tb_ap, TGB[ch, :, tsl], reads=[scr_tok["tgb"]], writes=[tb_t])
                    pa, pa_t = psb((ch % 2) * 2)
                    pbk, pbk_t = psb((ch % 2) * 2 + 1)
                    for kc in range(16):
                        mm(pa, wa_ap[:, kc, j * 128:(j + 1) * 128], ml_ap[:, kc, :], kc == 0, kc == 15, [wa_t, ml_t], [pa_t])
                    for kc in range(16):
                        mm(pbk, wb_ap[:, kc, j * 128:(j + 1) * 128], sw_ap[:, kc, :], kc == 0, kc == 15, [wb_t, sw_t], [pbk_t])
                    u_ap, u_t = uv.next()
                    v_ap, v_t = uv.next()
                    stt(u_ap, ta_ap, 1.0, pa, ALU.add, ALU.mult, [ta_t, pa_t], [u_t])
                    stt(v_ap, tb_ap, 1.0, pbk, ALU.add, ALU.mult, [tb_t, pbk_t], [v_t])
                    m_ap, m_t = mxo.next()
                    tt("pool", m_ap, u_ap, v_ap, ALU.add, [u_t, v_t], [m_t])
                    P.dma(MIXT[ch, :, tsl], m_ap, reads=[m_t], writes=[mixt_tok], eng="pool")
        P.barrier()
        A.reset()
        if dbg:
            for i in range(16):
                P.dma(dbgs["mixt"][0][i], MIXT[i], writes=[T()])
            P.barrier()

        wo, wo_t = A.tile([128, 4, 16, 512], BF16)
        for s in range(4):
            P.dma(wo[:, s], WS["out"][s], reads=[ws_tok["out"]], writes=[wo_t])
        wr, wr_t = A.tile([128, 16, 32], F32)
        P.dma(wr, w_router.rearrange("(k p) e -> p k e", p=128), writes=[wr_t])
        tt("dve", wr, wr, rs[:, 1, :].unsqueeze(2).to_broadcast([128, 16, 32]), ALU.mult, [wr_t, rs_t], [wr_t])
        brb, brb_t = A.tile([128, 32], F32)
        P.dma(brb, b_router.partition_broadcast(128), writes=[brb_t])
        eoff, eoff_t = A.tile([128, 32], F32)
        P.op("pool", lambda e: e.iota(eoff, pattern=[[CAP, 32]], base=0, channel_multiplier=0, allow_small_or_imprecise_dtypes=True), (), [eoff_t])
        cntB, cntB_t = A.tile([128, 32], F32)
        memset("pool", cntB, 0.0, [cntB_t])
        mxs = A.ring(2, [128, 16, 512], BF16)
        xin = A.ring(1, [128, D], F32)
        h1r = A.ring(2, [128, D], F32)
        xnf = A.ring(1, [128, D], F32)
        xnb = A.ring(2, [128, D], BF16)
        xTf = A.ring(1, [128, 16, 128], F32)
        sm = A.ring(4, [128, 4], F32)
        rt = A.ring(2, [128, 16, 32], F32)
        t8 = A.ring(2, [128, 8], F32)
        h1_tok, xg_tok = T(), T()
        for tix in range(32):
            if tix % 4 == 0:
                mx_ap, mx_t = mxs.next()
                P.dma(mx_ap, MIXT[:, :, tix * 128:tix * 128 + 512].rearrange("k p c -> p k c"), reads=[mixt_tok], writes=[mx_t])
            c0 = (tix % 4) * 128
            x_ap, x_t = xin.next()
            P.dma(x_ap, xall[NPRE + tix * 128:NPRE + (tix + 1) * 128, :], writes=[x_t])
            h_ap, h_t = h1r.next()
            for s in range(4):
                p_ap, p_t = psb(s % 2)
                for kc in range(16):
                    mm(p_ap, mx_ap[:, kc, c0:c0 + 128], wo[:, s, kc, :], kc == 0, kc == 15, [mx_t, wo_t], [p_t])
                tt("dve", h_ap[:, s * 512:(s + 1) * 512], p_ap, x_ap[:, s * 512:(s + 1) * 512], ALU.add, [p_t, x_t], [h_t])
            P.dma(H1[tix * 128:(tix + 1) * 128, :], h_ap, reads=[h_t], writes=[h1_tok], eng="pool")
            if dbg:
                P.dma(dbgs["h1"][tix * 128:(tix + 1) * 128, :], h_ap, reads=[h_t], writes=[T()])
            s_ap, s_t = sm.next()
            act(junk, h_ap, AF.Square, [h_t], [junk_t, s_t], accum=s_ap[:, 0:1])
            rsqrt_act(s_ap[:, 2:3], s_ap[:, 0:1], [s_t], [s_t], scale=1.0 / D, eps=True)
            xb_ap, xb_t = xnb.next()
            act(xb_ap, h_ap, AF.Copy, [h_t, s_t], [xb_t], scale=s_ap[:, 2:3])
            xf_ap, xf_t = xnf.next()
            act(xf_ap, h_ap, AF.Copy, [h_t, s_t], [xf_t], scale=s_ap[:, 2:3])
            xT_ap, xT_t = xTf.next()
            for q4 in range(4):
                pt_, pt_t = psb(2 + q4 % 2)
                for k in range(4):
                    kc = q4 * 4 + k
                    tr(pt_[:, k * 128:(k + 1) * 128], xf_ap[:, kc * 128:(kc + 1) * 128], IDF, [xf_t, cf_t], [pt_t])
                cp("act", xT_ap[:, q4 * 4:(q4 + 1) * 4, :], pt_.rearrange("p (k c) -> p k c", c=128), [pt_t], [xT_t])
            pl, pl_t = psb(4)
            for kc in range(16):
                mm(pl[:, 0:32], xT_ap[:, kc, :], wr[:, kc, :], kc == 0, kc == 15, [xT_t, wr_t], [pl_t])
            r_ap, r_t = rt.next()
            lg = r_ap[:, 0, :]
            tt("dve", lg, pl[:, 0:32], brb, ALU.add, [pl_t, brb_t], [r_t])
            t_ap, t_t = t8.next()
            P.op("dve", lambda e, t_ap=t_ap, lg=lg: e.max(out=t_ap, in_=lg), [r_t], [t_t])
            ts("dve", t_ap[:, 4:8], t_ap[:, 0:4], t_ap[:, 0:1], None, ALU.subtract, None, [t_t], [t_t])
            act(t_ap[:, 4:8], t_ap[:, 4:8], AF.Exp, [t_t], [t_t])
            P.op("dve", lambda e, t_ap=t_ap, s_ap=s_ap: e.reduce_sum(out=s_ap[:, 3:4], in_=t_ap[:, 4:8], axis=AX.X), [t_t], [s_t])
            P.op("dve", lambda e, s_ap=s_ap: e.reciprocal(out=s_ap[:, 3:4], in_=s_ap[:, 3:4]), [s_t], [s_t])
            ts("dve", t_ap[:, 4:8], t_ap[:, 4:8], s_ap[:, 3:4], None, ALU.mult, None, [t_t, s_t], [t_t])
            msk = r_ap[:, 1, :]
            ts("dve", msk, lg, t_ap[:, 3:4], None, ALU.is_ge, None, [r_t, t_t], [r_t])
            mb = r_ap[:, 2, :].bitcast(BF16)[:, 0:32]
            cp("dve", mb, msk, [r_t], [r_t])
            pp, pp_t = psb(5)
            mm(pp[:, 0:32], TRIS, mb, True, True, [cb_t, r_t], [pp_t])
            pos = r_ap[:, 3, :]
            tt("dve", pos, pp[:, 0:32], cntB, ALU.add, [pp_t, cntB_t], [r_t])
            pc2, pc2_t = psb(7)
            mm(pc2[:, 0:32], ONESF, msk, True, True, [cf_t, r_t], [pc2_t])
            tt("dve", cntB, cntB, pc2[:, 0:32], ALU.add, [pc2_t, cntB_t], [cntB_t])
            for k in range(4):
                oh = r_ap[:, 4 + k, :]
                ts("dve", oh, lg, t_ap[:, k:k + 1], None, ALU.is_equal, None, [r_t, t_t], [r_t])
                pk = r_ap[:, 8 + k, :]
                tt("dve", pk, oh, pos, ALU.mult, [r_t], [r_t])
                P.op("dve", lambda e, pk=pk, r_ap=r_ap, k=k: e.reduce_sum(out=r_ap[:, 12, k:k + 1], in_=pk, axis=AX.X), [r_t], [r_t])
                tt("dve", pk, oh, eoff, ALU.mult, [r_t, eoff_t], [r_t])
                P.op("dve", lambda e, pk=pk, r_ap=r_ap, k=k: e.reduce_sum(out=r_ap[:, 12, 4 + k:5 + k], in_=pk, axis=AX.X), [r_t], [r_t])
            okk = r_ap[:, 12, 8:12]
            ts("dve", okk, r_ap[:, 12, 0:4], float(CAP), None, ALU.is_lt, None, [r_t], [r_t])
            slf = r_ap[:, 12, 12:16]
            tt("dve", slf, r_ap[:, 12, 0:4], r_ap[:, 12, 4:8], ALU.add, [r_t], [r_t])
            ts("dve", slf, slf, float(TRASH), None, ALU.subtract, None, [r_t], [r_t])
            tt("dve", slf, slf, okk, ALU.mult, [r_t], [r_t])
            ts("dve", slf, slf, float(TRASH), None, ALU.add, None, [r_t], [r_t])
            cp("dve", SLOT[:, tix, :], slf, [r_t], [SLOT_t])
            tt("dve", GATE[:, tix, :], t_ap[:, 4:8], okk, ALU.mult, [t_t, r_t], [GATE_t])
            for k in range(4):
                P.op("pool", lambda e, k=k, tix=tix, xb_ap=xb_ap: e.indirect_dma_start(
                    out=XG, out_offset=bass.IndirectOffsetOnAxis(ap=SLOT[:, tix, k:k + 1], axis=0), in_=xb_ap, in_offset=None),
                    [xb_t, SLOT_t], [xg_tok], dma=True)
        P.barrier()
        A.reset()

        stg = A.ring(2, [128, 16, 512], F32)
        wbf = A.ring(2, [128, 16, 512], BF16)
        xgl = A.ring(4, [128, D], BF16)
        xgT, xgT_t = A.tile([128, 16, CAP], BF16)
        actT, actT_t = A.tile([128, 16, CAP], BF16)
        bupr = A.ring(2, [128, 32], F32)
        bdb = A.ring(2, [1, D], BF16)
        ones1, ones1_t = A.tile([1, 128], BF16)
        memset("pool", ones1, 1.0, [ones1_t])
        gl = A.ring(2, [128, CAP // 2], F32)
        sg = A.ring(2, [128, CAP // 2], F32)
        l1 = A.ring(2, [128, CAP // 2], F32)
        yo = A.ring(3, [128, 512], F32)
        ys_tok = T()
        zrow, zrow_t = A.tile([128, 512], F32)
        memset("pool", zrow, 0.0, [zrow_t])
        for s in range(4):
            P.dma(YS[NSLOT:NSLOT + 128, s * 512:(s + 1) * 512], zrow, reads=[zrow_t], writes=[ys_tok])
        NH = CAP // 2
        estate = {}

        def prologue(e_):
            for r0, nr in ETILES:
                g_ap, g_t = xgl.next()
                P.dma(g_ap[0:nr, :], XG[e_ * CAP + r0:e_ * CAP + r0 + nr, :], reads=[xg_tok], writes=[g_t])
                for half in range(2):
                    pb, pb_t = psb(6 + half, BF16)
                    for k in range(8):
                        kc = half * 8 + k
                        tr(pb[:, k * 128:k * 128 + nr], g_ap[0:nr, kc * 128:(kc + 1) * 128], IDB[0:nr, 0:nr], [g_t, cb_t], [pb_t])
                    cp("act" if half else "dve", xgT[:, half * 8:(half + 1) * 8, r0:r0 + nr],
                       pb[:, 0:1024].rearrange("p (k c) -> p k c", c=128)[:, :, 0:nr], [pb_t], [xgT_t])
            bup, bup_t = bupr.next()
            P.dma(bup, b_up[e_].rearrange("(c p) -> p c", p=128), writes=[bup_t], allow_slow_non_contiguous=True)
            bb_ap, bb_t = bdb.next()
            P.dma(bb_ap, b_dn[e_:e_ + 1, :], writes=[bb_t], eng="pool")
            estate[e_] = (bup, bup_t, bb_ap, bb_t)

        def up_task(e_, jj):
            st_ = {}

            def load():
                s_ap, s_t = stg.next()
                P.dma(s_ap[:, :, 0:256], w_up[e_, :, jj * 256:(jj + 1) * 256].rearrange("(k p) c -> p k c", p=128), writes=[s_t])
                P.dma(s_ap[:, :, 256:512], w_up[e_, :, D + jj * 256:D + (jj + 1) * 256].rearrange("(k p) c -> p k c", p=128), writes=[s_t])
                st_["s"] = (s_ap, s_t)

            def cast():
                s_ap, s_t = st_["s"]
                w_ap, w_t = wbf.next()
                for kc in range(16):
                    cast_scale(w_ap[:, kc, :], s_ap[:, kc, :], rs[:, 1, kc:kc + 1], [s_t, rs_t], [w_t])
                st_["w"] = (w_ap, w_t)

            def compute():
                w_ap, w_t = st_["w"]
                bup, bup_t, _, _ = estate[e_]
                for hf in range(2):
                    ch = jj * 2 + hf
                    for nh in range(2):
                        pg, pg_t = psb(nh * 2)
                        plin, plin_t = psb(nh * 2 + 1)
                        for kc in range(16):
                            mm(pg[:, 0:NH], w_ap[:, kc, hf * 128:(hf + 1) * 128], xgT[:, kc, nh * NH:(nh + 1) * NH], kc == 0, kc == 15, [w_t, xgT_t], [pg_t])
                        for kc in range(16):
                            mm(plin[:, 0:NH], w_ap[:, kc, 256 + hf * 128:256 + (hf + 1) * 128], xgT[:, kc, nh * NH:(nh + 1) * NH], kc == 0, kc == 15, [w_t, xgT_t], [plin_t])
                        g_ap, g_t = gl.next()
                        ts("dve", g_ap, pg[:, 0:NH], bup[:, ch:ch + 1], 7.0, ALU.add, ALU.min, [pg_t, bup_t], [g_t])
                        sg_ap, sg_t = sg.next()
                        act(sg_ap, g_ap, AF.Tanh, [g_t], [sg_t], scale=0.851)
                        l_ap, l_t = l1.next()
                        ts("dve", l_ap, plin[:, 0:NH], bup[:, 16 + ch:17 + ch], 7.0, ALU.add, ALU.min, [plin_t, bup_t], [l_t])
                        ts("dve", l_ap, l_ap, -7.0, 1.0, ALU.max, ALU.add, [l_t], [l_t])
                        stt(sg_ap, sg_ap, 1.0, g_ap, ALU.add, ALU.mult, [sg_t, g_t], [sg_t])
                        tt("dve", actT[:, ch, nh * NH:(nh + 1) * NH], sg_ap, l_ap, ALU.mult, [sg_t, l_t], [actT_t])
            return load, cast, compute

        def down_task(e_, s):
            st_ = {}

            def load():
                s_ap, s_t = stg.next()
                P.dma(s_ap, w_dn[e_, :, s * 512:(s + 1) * 512].rearrange("(k p) c -> p k c", p=128), writes=[s_t])
                st_["s"] = (s_ap, s_t)

            def cast():
                s_ap, s_t = st_["s"]
                w_ap, w_t = wbf.next()
                for kc in range(16):
                    cast_scale(w_ap[:, kc, :], s_ap[:, kc, :], rsh[:, 0:1], [s_t, half_t], [w_t])
                st_["w"] = (w_ap, w_t)

            def compute():
                w_ap, w_t = st_["w"]
                _, _, bb_ap, bb_t = estate[e_]
                for tt_, (r0, nr) in enumerate(ETILES):
                    pd, pd_t = psb(4 + tt_ % 2)
                    for kc in range(16):
                        mm(pd[0:nr, :], actT[:, kc, r0:r0 + nr], w_ap[:, kc, :], kc == 0, False, [actT_t, w_t], [pd_t])
                    mm(pd[0:nr, :], ones1[:, 0:nr], bb_ap[:, s * 512:(s + 1) * 512], False, True, [ones1_t, bb_t], [pd_t])
                    y_ap, y_t = yo.next()
                    cp("act", y_ap[0:nr, :], pd[0:nr, :], [pd_t], [y_t])
                    P.dma(YS[e_ * CAP + r0:e_ * CAP + r0 + nr, s * 512:(s + 1) * 512], y_ap[0:nr, :], reads=[y_t], writes=[ys_tok], eng="pool")
                if s == 0 and e_ + 1 < NE:
                    prologue(e_ + 1)
            return load, cast, compute

        tasks = []
        for e_ in range(NE):
            for jj in range(8):
                tasks.append(up_task(e_, jj))
            for s in range(4):
                tasks.append(down_task(e_, s))
        prologue(0)
        tasks[0][0]()
        tasks[1][0]()
        tasks[0][1]()
        for i in range(len(tasks)):
            if i + 2 < len(tasks):
                tasks[i + 2][0]()
            if i + 1 < len(tasks):
                tasks[i + 1][1]()
            tasks[i][2]()
        P.barrier()
        A.reset()

        wpg, wpg_t = A.tile([128, 4, 16, 512], BF16)
        for s in range(4):
            P.dma(wpg[:, s], WS["pg"][s], reads=[ws_tok["pg"]], writes=[wpg_t])
        wpp, wpp_t = A.tile([128, 2, D], BF16)
        for kc in range(2):
            s_ap, s_t = A.tile([128, D], F32)
            P.dma(s_ap, w_pp[kc * 128:(kc + 1) * 128, :], writes=[s_t])
            cp("act", wpp[:, kc, :], s_ap, [s_t], [wpp_t])
        h2r = A.ring(2, [128, D], F32)
        ysr = A.ring(4, [128, D], F32)
        sm = A.ring(4, [128, 4], F32)
        xnb = A.ring(2, [128, D], BF16)
        xT3 = A.ring(2, [128, 16, 128], BF16)
        pin = A.ring(2, [128, 256], F32)
        pbf = A.ring(2, [128, 256], BF16)
        pT = A.ring(2, [128, 2, 128], BF16)
        tgr = A.ring(2, [128, 512], F32)
        tmr = A.ring(2, [128, 512], F32)
        outr = A.ring(2, [128, D], F32)
        out_tok = T()
        for tix in range(32):
            h_ap, h_t = h2r.next()
            P.dma(h_ap, H1[tix * 128:(tix + 1) * 128, :], reads=[h1_tok], writes=[h_t])
            for k in range(4):
                y_ap, y_t = ysr.next()
                P.op("pool", lambda e, k=k, tix=tix, y_ap=y_ap: e.indirect_dma_start(
                    out=y_ap, out_offset=None, in_=YS, in_offset=bass.IndirectOffsetOnAxis(ap=SLOT[:, tix, k:k + 1], axis=0)),
                    [ys_tok, SLOT_t], [y_t], dma=True)
                stt(h_ap, y_ap, GATE[:, tix, k:k + 1], h_ap, ALU.mult, ALU.add, [y_t, GATE_t, h_t], [h_t])
            if dbg:
                P.dma(dbgs["h2"][tix * 128:(tix + 1) * 128, :], h_ap, reads=[h_t], writes=[T()])
            s_ap, s_t = sm.next()
            act(junk, h_ap, AF.Square, [h_t], [junk_t, s_t], accum=s_ap[:, 0:1])
            rsqrt_act(s_ap[:, 2:3], s_ap[:, 0:1], [s_t], [s_t], scale=1.0 / D, eps=True)
            xb_ap, xb_t = xnb.next()
            act(xb_ap, h_ap, AF.Copy, [h_t, s_t], [xb_t], scale=s_ap[:, 2:3])
            xT_ap, xT_t = xT3.next()
            for half in range(2):
                pb, pb_t = psb(6 + half, BF16)
                for k in range(8):
                    kc = half * 8 + k
                    tr(pb[:, k * 128:(k + 1) * 128], xb_ap[:, kc * 128:(kc + 1) * 128], IDB, [xb_t, cb_t], [pb_t])
                cp("act", xT_ap[:, half * 8:(half + 1) * 8, :], pb[:, 0:1024].rearrange("p (k c) -> p k c", c=128), [pb_t], [xT_t])
            pi_ap, pi_t = pin.next()
            P.dma(pi_ap, p_in[tix * 128:(tix + 1) * 128, :], writes=[pi_t])
            pb_ap, pbt = pbf.next()
            cp("pool", pb_ap, pi_ap, [pi_t], [pbt])
            pT_ap, pT_t = pT.next()
            pq, pq_t = psb(5, BF16)
            for kc in range(2):
                tr(pq[:, kc * 128:(kc + 1) * 128], pb_ap[:, kc * 128:(kc + 1) * 128], IDB, [pbt, cb_t], [pq_t])
            cp("dve", pT_ap, pq[:, 0:256].rearrange("p (k c) -> p k c", c=128), [pq_t], [pT_t])
            o_ap, o_t = outr.next()
            for s in range(4):
                pgt, pgt_t = psb(s % 2)
                for kc in range(16):
                    mm(pgt, xT_ap[:, kc, :], wpg[:, s, kc, :], kc == 0, kc == 15, [xT_t, wpg_t], [pgt_t])
                ppe, ppe_t = psb(2 + s % 2)
                for kc in range(2):
                    mm(ppe, pT_ap[:, kc, :], wpp[:, kc, s * 512:(s + 1) * 512], kc == 0, kc == 1, [pT_t, wpp_t], [ppe_t])
                tg_ap, tg_t = tgr.next()
                act(tg_ap, pgt, AF.Tanh, [pgt_t], [tg_t], scale=0.5)
                tm_ap, tm_t = tmr.next()
                stt(tm_ap, tg_ap, 1.0, ppe, ALU.add, ALU.mult, [tg_t, ppe_t], [tm_t])
                stt(o_ap[:, s * 512:(s + 1) * 512], tm_ap, 0.5, h_ap[:, s * 512:(s + 1) * 512], ALU.mult, ALU.add, [tm_t, h_t], [o_t])
            P.dma(y_out[tix * 128:(tix + 1) * 128, :], o_ap, reads=[o_t], writes=[out_tok])
        P.op("sp", lambda e: e.nop(), [out_tok], [])
        P.barrier()

        run = P.emit(sems, dsems)

        @block.sync
        def _(e):
            run("sp", e)

        @block.scalar
        def _(e):
            run("act", e)

        @block.vector
        def _(e):
            run("dve", e)

        @block.gpsimd
        def _(e):
            run("pool", e)

        @block.tensor
        def _(e):
            run("pe", e)
    return nc


def _consts():
    i = np.arange(128)
    ident = np.eye(128, dtype=np.float32)
    tri = (i[:, None] <= i[None, :]).astype(np.float32)
    ones = np.ones((128, 128), np.float32)
    cst_f = np.stack([ident, tri, ones], axis=1)
    negm = np.where(i[None, :] < i[:, None], -30000.0, 0.0).astype(np.float32)
    bd = (i[:, None] // 64 == i[None, :] // 64).astype(np.float32) / 64.0
    tris = (i[:, None] < i[None, :]).astype(np.float32)
    cst_b = np.stack([ident, negm, bd, tris], axis=1).astype(ml_dtypes.bfloat16)
    slopes = 2.0 ** (-8.0 * np.arange(1, 33, dtype=np.float64) / 32)
    eb = np.zeros((128, 4, 2, 2, 4, 128), np.float64)
    q = np.arange(128)
    for g in range(4):
        for kb in range(2):
            kpos = np.arange(128) + 128 * kb
            dist = (q[None, :] + 128) - kpos[:, None]
            win = (dist >= 0) & (dist < 128)
            for ab in range(2):
                for j in range(4):
                    h = 8 * g + 2 * j + ab
                    eb[:, g, kb, ab, j, :] = np.where(win, np.exp(-slopes[h] * dist), 0.0)
    ebm = eb.reshape(128, -1).astype(ml_dtypes.bfloat16)
    return cst_f, cst_b, ebm


_NC_CACHE = {}


def kernel(x, p, norm_mix_w, w_in, ml_igate_b, ml_fgate_b, ml_norm_w, sw_q_norm_w, sw_k_norm_w, sw_sinks,
           w_branch_a, w_branch_b, w_out, norm_ffn_w, w_router, b_router, w_expert_up, b_expert_up,
           w_expert_down, b_expert_down, norm_ple_w, w_ple_gate, w_ple_proj, _dbg=False, _ncores=8):
    f = lambda a: np.ascontiguousarray(np.asarray(a, dtype=np.float32))
    x = f(x)
    p = f(p)[0]
    cst_f, cst_b, ebm = _consts()
    shared = {
        "cst_f": cst_f, "cst_b": cst_b, "ebm": ebm,
        "w_in": f(w_in)[0], "norm_mix_w": f(norm_mix_w)[0], "ml_igate_b": f(ml_igate_b)[0], "ml_fgate_b": f(ml_fgate_b)[0],
        "ml_norm_w": f(ml_norm_w)[0], "sw_q_norm_w": f(sw_q_norm_w)[0], "sw_k_norm_w": f(sw_k_norm_w)[0],
        "sw_sinks": f(sw_sinks)[0], "w_branch_a": f(w_branch_a)[0], "w_branch_b": f(w_branch_b)[0], "w_out": f(w_out)[0],
        "norm_ffn_w": f(norm_ffn_w)[0], "w_router": f(w_router)[0], "b_router": f(b_router)[0],
        "w_expert_up": f(w_expert_up)[0], "b_expert_up": f(b_expert_up)[0], "w_expert_down": f(w_expert_down)[0],
        "b_expert_down": f(b_expert_down)[0], "norm_ple_w": f(norm_ple_w)[0], "w_ple_gate": f(w_ple_gate)[0],
        "w_ple_proj": f(w_ple_proj)[0],
    }
    in_maps = []
    for c in range(_ncores):
        b, hf = c // 2, c % 2
        xall = np.zeros((NT_ALL, D), np.float32)
        if hf == 1:
            xall[:NPRE] = x[b, TOK - NPRE:TOK]
        xall[NPRE:] = x[b, hf * TOK:(hf + 1) * TOK]
        m = dict(shared)
        m["xall"] = xall
        m["p_own"] = np.ascontiguousarray(p[b, hf * TOK:(hf + 1) * TOK])
        m["flag"] = np.full((128, 1), float(hf), np.float32)
        in_maps.append(m)
    key = bool(_dbg)
    if key not in _NC_CACHE:
        _NC_CACHE[key] = build_nc(dbg=_dbg)
    nc = _NC_CACHE[key]
    res = run_bass_kernel_spmd(nc, in_maps, core_ids=list(range(_ncores)))
    out = np.zeros((4, 2 * TOK, D), np.float32)
    for c in range(_ncores):
        out[c // 2, (c % 2) * TOK:(c % 2 + 1) * TOK] = res.results[c]["y"]
    if _dbg:
        return out, res.results
    return out
```

```python
import numpy as np
import ml_dtypes
from contextlib import ExitStack
import concourse.bass as bass
import concourse.mybir as mybir
from concourse.bass_utils import run_bass_kernel_spmd

F32 = mybir.dt.float32
BF16 = mybir.dt.bfloat16
I32 = mybir.dt.int32
U8 = mybir.dt.uint8
AF = mybir.ActivationFunctionType
ALU = mybir.AluOpType
AX = mybir.AxisListType

ENGS = ("pe", "act", "dve", "pool", "sp")
NDMASEM = 24

D = 2048
TOK = 4096
NPRE = 4096
NT_ALL = NPRE + TOK
NE = 32
CAP = 704
ETILES = [(r, min(128, CAP - r)) for r in range(0, CAP, 128)]
NSLOT = NE * CAP
TRASH = NSLOT
EPS = 1e-6
ARENA_BYTES = 200 * 1024


class Tok:
    __slots__ = ("w", "r")

    def __init__(self):
        self.w = None
        self.r = []


class Prog:
    def __init__(self):
        self.ops, self.deps, self.eng_of, self.isdma = [], [], [], []
        self.since_barrier = []

    def op(self, eng, fn, reads=(), writes=(), dma=False):
        i = len(self.ops)
        d = set()
        for t in reads:
            if t.w is not None:
                d.add(t.w)
        for t in writes:
            if t.w is not None:
                d.add(t.w)
            d.update(t.r)
        for t in reads:
            t.r.append(i)
        for t in writes:
            t.w = i
            t.r = []
        self.ops.append(fn)
        self.deps.append(d)
        self.eng_of.append(eng)
        self.isdma.append(dma)
        if dma:
            self.since_barrier.append(i)
        return i

    def dma(self, out, in_, reads=(), writes=(), eng="sp", **kw):
        return self.op(eng, lambda e: e.dma_start(out=out, in_=in_, **kw), reads, writes, dma=True)

    def barrier(self):
        last = {}
        for i, e in enumerate(self.eng_of):
            last[e] = i
        i0 = self.op("sp", lambda e: e.nop())
        self.deps[i0].update(last.values())
        self.deps[i0].update(self.since_barrier)
        self.since_barrier = []
        for e in ENGS:
            if e == "sp":
                continue
            j = self.op(e, lambda en: en.nop())
            self.deps[j].add(i0)

    def emit(self, sems, dma_sems):
        n = len(self.ops)
        has_dep = [False] * n
        for i in range(n):
            ei = self.eng_of[i]
            for d in self.deps[i]:
                if self.eng_of[d] == "pe" and ei == "pe" and not self.isdma[d]:
                    continue
                has_dep[d] = True
        sig = [None] * n
        cnt = {e: 0 for e in ENGS}
        dcnt = {e: 0 for e in ENGS}
        dma_prev = {}
        ring_guard = [None] * n
        for i in range(n):
            e = self.eng_of[i]
            if self.isdma[i]:
                k = dcnt[e]
                dcnt[e] += 1
                slot, rnd = k % NDMASEM, k // NDMASEM
                sig[i] = (dma_sems[e][slot], 16 * (rnd + 1))
                if (e, slot) in dma_prev:
                    ring_guard[i] = dma_prev[(e, slot)]
                dma_prev[(e, slot)] = i
            elif has_dep[i]:
                cnt[e] += 1
                sig[i] = (sems[e], cnt[e])
        per_eng = {e: [] for e in ENGS}
        for i in range(n):
            per_eng[self.eng_of[i]].append(i)

        def run(e, engobj):
            waited = {}
            for i in per_eng[e]:
                need = {}
                dl = list(self.deps[i])
                if ring_guard[i] is not None:
                    dl.append(ring_guard[i])
                for d in dl:
                    if self.eng_of[d] == "pe" and e == "pe" and not self.isdma[d]:
                        continue
                    s, v = sig[d]
                    key = id(s)
                    if waited.get(key, 0) >= v:
                        continue
                    if key not in need or need[key][1] < v:
                        need[key] = (s, v)
                for key, (s, v) in need.items():
                    engobj.wait_ge(s, v)
                    waited[key] = v
                ins = self.ops[i](engobj)
                if sig[i] is not None:
                    ins.then_inc(sig[i][0], 16 if self.isdma[i] else 1)

        return run


class Arena:
    def __init__(self, ar):
        self.ar = ar
        self.base = 0
        self.off = 0

    def alloc(self, shape, dt, persist=False):
        esz = 4 if dt in (F32, I32) else 2
        n = int(np.prod(shape[1:])) * esz
        n = (n + 63) // 64 * 64
        a = self.off
        self.off += n
        assert self.off <= ARENA_BYTES, f"SBUF arena overflow {self.off}"
        if persist:
            self.base = self.off
        ap = self.ar[0:shape[0], a:a + int(np.prod(shape[1:])) * esz].bitcast(dt)
        if len(shape) > 2:
            names = "abcdefg"[:len(shape) - 1]
            kw = {names[i]: shape[i + 1] for i in range(1, len(names))}
            ap = ap.rearrange("p (" + " ".join(names) + ") -> p " + " ".join(names), **kw)
        return ap

    def tile(self, shape, dt, persist=False):
        return (self.alloc(shape, dt, persist), Tok())

    def ring(self, n, shape, dt):
        return Ring([self.tile(shape, dt) for _ in range(n)])

    def reset(self):
        self.off = self.base


class Ring:
    def __init__(self, items):
        self.items = items
        self.i = 0

    def next(self):
        it = self.items[self.i % len(self.items)]
        self.i += 1
        return it


def build_nc(dbg=False):
    nc = bass.Bass("TRN2", target_bir_lowering=False)

    def din(name, shape, dt=F32):
        return nc.dram_tensor(name, list(shape), dt, kind="ExternalInput").ap()

    def dscr(name, shape, dt):
        return nc.dram_tensor(name, list(shape), dt, kind="Internal").ap()

    xall = din("xall", [NT_ALL, D])
    p_in = din("p_own", [TOK, 256])
    flag_in = din("flag", [128, 1])
    cst_f = din("cst_f", [128, 3, 128])
    cst_b = din("cst_b", [128, 4, 128], BF16)
    ebm_in = din("ebm", [128, 4 * 2 * 2 * 512], BF16)
    w_in = din("w_in", [D, 12816])
    norm_mix_w = din("norm_mix_w", [D])
    ml_ib = din("ml_igate_b", [8])
    ml_fb = din("ml_fgate_b", [8])
    ml_nw = din("ml_norm_w", [D])
    sw_qn = din("sw_q_norm_w", [64])
    sw_kn = din("sw_k_norm_w", [64])
    sw_sinks = din("sw_sinks", [32])
    w_a = din("w_branch_a", [D, D])
    w_b = din("w_branch_b", [D, D])
    w_out = din("w_out", [D, D])
    norm_ffn_w = din("norm_ffn_w", [D])
    w_router = din("w_router", [D, NE])
    b_router = din("b_router", [NE])
    w_up = din("w_expert_up", [NE, D, 2 * D])
    b_up = din("b_expert_up", [NE, 2 * D])
    w_dn = din("w_expert_down", [NE, D, D])
    b_dn = din("b_expert_down", [NE, D])
    norm_ple_w = din("norm_ple_w", [D])
    w_pg = din("w_ple_gate", [D, D])
    w_pp = din("w_ple_proj", [256, D])
    y_out = nc.dram_tensor("y", [TOK, D], F32, kind="ExternalOutput").ap()

    WS = {}
    for name, ns in (("qk", 4), ("kv", 6), ("o", 4), ("sq", 4), ("sk", 2), ("ga", 4), ("gb", 4),
                     ("a", 4), ("b", 4), ("out", 4), ("pg", 4)):
        WS[name] = dscr("ws_" + name, [ns, 128, 16, 512], BF16)
    WS_sv = dscr("ws_sv", [128, 16, 256], BF16)
    WS_g = dscr("ws_g", [128, 16, 16], BF16)
    MQT = dscr("mqt", [8, 128, TOK], BF16)
    MKT = dscr("mkt", [8, 128, TOK], BF16)
    MK = dscr("mk", [NT_ALL, 1024], BF16)
    MV = dscr("mv", [NT_ALL, 2048], BF16)
    MO = dscr("mo", [TOK, 2048], BF16)
    SQ = dscr("sq", [16, 128, TOK], BF16)
    SK = dscr("sk", [8, 128, TOK + 512], BF16)
    SV = dscr("sv", [TOK + 512, 256], BF16)
    TGA = dscr("tga", [16, 128, TOK], BF16)
    TGB = dscr("tgb", [16, 128, TOK], BF16)
    MLT = dscr("mlt", [16, 128, TOK], BF16)
    SWT = dscr("swt", [16, 128, TOK], BF16)
    MIXT = dscr("mixt", [16, 128, TOK], BF16)
    H1 = dscr("h1", [TOK, D], F32)
    XG = dscr("xg", [NSLOT + 128, D], BF16)
    YS = dscr("ys", [NSLOT + 128, D], F32)
    dbgs = {}
    if dbg:
        dbgs["h1"] = nc.dram_tensor("dbg_h1", [TOK, D], F32, kind="ExternalOutput").ap()
        dbgs["mlt"] = nc.dram_tensor("dbg_mlt", [16, 128, TOK], BF16, kind="ExternalOutput").ap()
        dbgs["swt"] = nc.dram_tensor("dbg_swt", [16, 128, TOK], BF16, kind="ExternalOutput").ap()
        dbgs["h2"] = nc.dram_tensor("dbg_h2", [TOK, D], F32, kind="ExternalOutput").ap()
        for nm, src in (("mk", MK), ("mv", MV), ("mo", MO), ("mqt", MQT), ("mkt", MKT), ("sq", SQ), ("sk", SK), ("sv", SV),
                        ("tga", TGA), ("mixt", MIXT)):
            dbgs[nm] = (nc.dram_tensor("dbg_" + nm, list(src.shape), BF16, kind="ExternalOutput").ap(), src)
        dbgs["gp"] = nc.dram_tensor("dbg_gp", [128, NT_ALL // 128 * 16], F32, kind="ExternalOutput").ap()

    P = Prog()
    with ExitStack() as es:
        arena_t = es.enter_context(nc.sbuf_tensor("arena", [128, ARENA_BYTES], U8))
        ps_t = es.enter_context(nc.psum_tensor("ps", [128, 8, 512], F32))
        sems = {e: es.enter_context(nc.semaphore("s_" + e)) for e in ENGS}
        dsems = {e: [es.enter_context(nc.semaphore(f"d_{e}{i}")) for i in range(NDMASEM)] for e in ("sp", "pool", "act")}
        block = es.enter_context(nc.Block())
        A = Arena(arena_t)
        PS = [(ps_t[:, b, :], Tok()) for b in range(8)]

        def psb(b, dt=F32):
            ap, t = PS[b]
            return (ap if dt == F32 else ap.bitcast(dt)), t

        T = lambda: Tok()

        def mm(out, lhsT, rhs, start, stop, reads, writes):
            P.op("pe", lambda e: e.matmul(out, lhsT, rhs, start=start, stop=stop), reads, writes)

        def tr(out, in_, ident, reads, writes):
            P.op("pe", lambda e: e.transpose(out, in_, ident), reads, writes)

        def act(out, in_, func, reads, writes, scale=1.0, bias=None, accum=None):
            kw = {}
            if bias is not None:
                kw["bias"] = bias
            if accum is not None:
                kw["accum_out"] = accum
            P.op("act", lambda e: e.activation(out=out, in_=in_, func=func, scale=scale, **kw), reads, writes)

        def ts(eng, out, in0, s1, s2, op0, op1, reads, writes):
            if op1 is None:
                P.op(eng, lambda e: e.tensor_scalar(out=out, in0=in0, scalar1=s1, scalar2=None, op0=op0), reads, writes)
            else:
                P.op(eng, lambda e: e.tensor_scalar(out=out, in0=in0, scalar1=s1, scalar2=s2, op0=op0, op1=op1), reads, writes)

        def tt(eng, out, in0, in1, op, reads, writes):
            P.op(eng, lambda e: e.tensor_tensor(out=out, in0=in0, in1=in1, op=op), reads, writes)

        def stt(out, in0, scalar, in1, op0, op1, reads, writes):
            P.op("dve", lambda e: e.scalar_tensor_tensor(out=out, in0=in0, scalar=scalar, in1=in1, op0=op0, op1=op1), reads, writes)

        def cp(eng, out, in_, reads, writes):
            if eng == "act":
                P.op("act", lambda e: e.copy(out=out, in_=in_), reads, writes)
            else:
                P.op(eng, lambda e: e.tensor_copy(out=out, in_=in_), reads, writes)

        def memset(eng, ap, val, writes):
            P.op(eng, lambda e: e.memset(ap, val), (), writes)

        cf, cf_t = A.tile([128, 3, 128], F32, persist=True)
        cb, cb_t = A.tile([128, 4, 128], BF16, persist=True)
        P.dma(cf, cst_f, writes=[cf_t])
        P.dma(cb, cst_b, writes=[cb_t])
        IDF, TRI, ONESF = cf[:, 0, :], cf[:, 1, :], cf[:, 2, :]
        IDB, NEGM, BD64, TRIS = cb[:, 0, :], cb[:, 1, :], cb[:, 2, :], cb[:, 3, :]
        neghalf, neghalf_t = A.tile([128, 512], F32, persist=True)
        memset("pool", neghalf, -0.5, [neghalf_t])
        flag, flag_t = A.tile([128, 1], F32, persist=True)
        P.dma(flag, flag_in, writes=[flag_t])
        NTILES = NT_ALL // 128
        GP, GP_t = A.tile([128, NTILES, 16], F32, persist=True)
        SLOT, SLOT_t = A.tile([128, 32, 4], I32, persist=True)
        GATE, GATE_t = A.tile([128, 32, 4], F32, persist=True)
        rs, rs_t = A.tile([128, 4, 16], F32, persist=True)
        for i, v in enumerate((norm_mix_w, norm_ffn_w, norm_ple_w)):
            P.dma(rs[:, i, :], v.rearrange("(k p) -> p k", p=128), writes=[rs_t], allow_slow_non_contiguous=True)
        half_t = T()
        junk, junk_t = A.tile([128, D], BF16, persist=True)
        rsh, _ = A.tile([128, 1], F32, persist=True)
        memset("pool", rsh, 0.5, [half_t])

        epsb, epsb_t = A.tile([128, 1], F32, persist=True)
        memset("pool", epsb, EPS, [epsb_t])

        def rsqrt_act(out, in_, reads, writes, scale=1.0, eps=False):
            if eps:
                act(out, in_, AF.Ln, list(reads) + [epsb_t], writes, scale=scale, bias=epsb[:, 0:1])
            else:
                act(out, in_, AF.Ln, reads, writes, scale=scale)
            act(out, out, AF.Exp, writes, writes, scale=-0.5)

        cast_i = [0]

        def cast_scale(out, in_, scale_ap, reads, writes, engs=("act", "dve", "act", "dve", "act", "dve", "act")):
            e = engs[cast_i[0] % len(engs)]
            cast_i[0] += 1
            if e == "act":
                if scale_ap is None:
                    P.op("act", lambda en: en.copy(out=out, in_=in_), reads, writes)
                else:
                    P.op("act", lambda en: en.activation(out=out, in_=in_, func=AF.Copy, scale=scale_ap), reads, writes)
            else:
                if scale_ap is None:
                    P.op(e, lambda en: en.tensor_copy(out=out, in_=in_), reads, writes)
                else:
                    P.op(e, lambda en: en.tensor_scalar(out=out, in0=in_, scalar1=scale_ap, scalar2=None, op0=ALU.mult), reads, writes)

        ws_tok = {k: T() for k in list(WS) + ["sv", "g"]}
        stg = A.ring(2, [128, 2048], F32)
        cst = A.ring(2, [128, 2048], BF16)
        kpad, kpad_t = A.tile([128, 1024], BF16)
        memset("pool", kpad, 0.0, [kpad_t])

        def cast_matrix(src, col0, ncols, dst, dst_tok, scale_idx, fscale=None, kchunks=16):
            for c0 in range(0, ncols, 2048):
                nc_ = min(2048, ncols - c0)
                for kc in range(kchunks):
                    s_ap, s_t = stg.next()
                    P.dma(s_ap[:, 0:nc_], src[kc * 128:(kc + 1) * 128, col0 + c0:col0 + c0 + nc_], writes=[s_t])
                    c_ap, c_t = cst.next()
                    if scale_idx is not None:
                        sc = rs[:, scale_idx, kc:kc + 1]
                        rd = [s_t, rs_t]
                    elif fscale is not None:
                        sc, rd = fscale, [s_t, half_t]
                    else:
                        sc, rd = None, [s_t]
                    cast_scale(c_ap[:, 0:nc_], s_ap[:, 0:nc_], sc, rd, [c_t], engs=("act", "dve"))
                    s0 = c0 // 512
                    ns = nc_ // 512
                    P.dma(dst[s0:s0 + ns, :, kc, :].rearrange("s p c -> p s c"),
                          c_ap[:, 0:nc_].rearrange("p (s c) -> p s c", c=512), reads=[c_t], writes=[dst_tok], eng="pool")

        C_Q, C_K, C_V, C_O, C_G, C_SQ, C_SK, C_SV, C_GA, C_GB = 0, 1024, 2048, 4096, 6144, 6160, 8208, 8464, 8720, 10768
        cast_matrix(w_in, C_Q, 2048, WS["qk"], ws_tok["qk"], 0)
        cast_matrix(w_in, C_K, 3072, WS["kv"], ws_tok["kv"], 0)
        cast_matrix(w_in, C_O, 2048, WS["o"], ws_tok["o"], 0)
        cast_matrix(w_in, C_SQ, 2048, WS["sq"], ws_tok["sq"], 0)
        cast_matrix(w_in, C_GA, 2048, WS["ga"], ws_tok["ga"], 0)
        cast_matrix(w_in, C_GB, 2048, WS["gb"], ws_tok["gb"], 0)
        cast_matrix(w_a, 0, 2048, WS["a"], ws_tok["a"], None)
        cast_matrix(w_b, 0, 2048, WS["b"], ws_tok["b"], None)
        cast_matrix(w_out, 0, 2048, WS["out"], ws_tok["out"], None, fscale=rsh[:, 0:1])
        cast_matrix(w_pg, 0, 2048, WS["pg"], ws_tok["pg"], 2)
        for kc in range(16):
            s_ap, s_t = stg.next()
            P.dma(s_ap[:, 0:16], w_in[kc * 128:(kc + 1) * 128, C_G:C_G + 16], writes=[s_t])
            P.dma(s_ap[:, 512:1024], w_in[kc * 128:(kc + 1) * 128, C_SK:C_SK + 512], writes=[s_t])
            c_ap, c_t = cst.next()
            sc = rs[:, 0, kc:kc + 1]
            ts("dve", c_ap[:, 0:16], s_ap[:, 0:16], sc, None, ALU.mult, None, [s_t, rs_t], [c_t])
            ts("dve", c_ap[:, 512:1024], s_ap[:, 512:1024], sc, None, ALU.mult, None, [s_t, rs_t], [c_t])
            P.dma(WS_g[:, kc, :], c_ap[:, 0:16], reads=[c_t], writes=[ws_tok["g"]])
            P.dma(WS_sv[:, kc, :], c_ap[:, 768:1024], reads=[c_t], writes=[ws_tok["sv"]])
            kp4 = kpad.rearrange("p (g ab c) -> p g ab c", ab=2, c=128)
            ksrc = c_ap[:, 512:768].rearrange("p (g c) -> p g c", c=64)
            cp("pool", kp4[:, :, 0, 0:64], ksrc, [c_t], [kpad_t])
            cp("pool", kp4[:, :, 1, 64:128], ksrc, [c_t], [kpad_t])
            P.dma(WS["sk"][0:2, :, kc, :].rearrange("s p c -> p s c"), kpad.rearrange("p (s c) -> p s c", c=512),
                  reads=[kpad_t], writes=[ws_tok["sk"]])
        P.barrier()
        A.reset()

        xin = A.ring(2, [128, D], F32)
        xnb = A.ring(2, [128, D], BF16)
        xnT = A.ring(2, [128, 16, 512], BF16)
        slab = A.ring(3, [128, 16, 512], BF16)
        sm = A.ring(4, [128, 4], F32)
        oev = A.ring(4, [128, 512], BF16)
        sqt = A.ring(2, [128, 512], BF16)
        r0 = A.ring(2, [128, 512], F32)
        r1 = A.ring(2, [128, 512], F32)
        wq8, wq8_t = A.tile([128, 2], F32)
        for hh in range(2):
            P.dma(wq8[hh * 64:(hh + 1) * 64, 0:1], sw_qn.rearrange("(p o) -> p o", o=1), writes=[wq8_t], allow_slow_non_contiguous=True)
            P.dma(wq8[hh * 64:(hh + 1) * 64, 1:2], sw_kn.rearrange("(p o) -> p o", o=1), writes=[wq8_t], allow_slow_non_contiguous=True)
        ts("dve", wq8[:, 0:1], wq8[:, 0:1], 0.125, None, ALU.mult, None, [wq8_t], [wq8_t])
        scr_tok = {k: T() for k in ("mqt", "mkt", "mk", "mv", "mo", "sq", "sk", "sv", "tga", "tgb")}
        evi = [0]
        psr = [0]

        def next_ps(lo=0, hi=6):
            b = lo + psr[0] % (hi - lo)
            psr[0] += 1
            return psb(b)

        def rms_rstd(x_ap, x_t, ncols):
            s_ap, s_t = sm.next()
            act(junk[:, 0:ncols], x_ap, AF.Square, [x_t], [junk_t, s_t], accum=s_ap[:, 0:1])
            rsqrt_act(s_ap[:, 2:3], s_ap[:, 0:1], [s_t], [s_t], scale=1.0 / ncols, eps=True)
            return s_ap[:, 2:3], s_t

        n_pre_st = NPRE // 512
        n_st = NT_ALL // 512
        for st in range(n_st):
            own = st >= n_pre_st
            halo = (st == n_pre_st - 1)
            ost = st - n_pre_st
            xt_ap, xt_t = xnT.next()
            for t4 in range(4):
                tix = st * 4 + t4
                x_ap, x_t = xin.next()
                P.dma(x_ap, xall[tix * 128:(tix + 1) * 128, :], writes=[x_t])
                rstd, rstd_t = rms_rstd(x_ap, x_t, D)
                xb_ap, xb_t = xnb.next()
                act(xb_ap, x_ap, AF.Copy, [x_t, rstd_t], [xb_t], scale=rstd)
                for half in range(2):
                    pb, pb_t = psb(6 + half, BF16)
                    for k in range(8):
                        kc = half * 8 + k
                        tr(pb[:, k * 128:(k + 1) * 128], xb_ap[:, kc * 128:(kc + 1) * 128], IDB, [xb_t, cb_t], [pb_t])
                    cp("dve" if half else "act", xt_ap[:, half * 8:(half + 1) * 8, t4 * 128:(t4 + 1) * 128],
                       pb[:, 0:1024].rearrange("p (k c) -> p k c", c=128), [pb_t], [xt_t])

            def fm_group(wname, sidx, nblk, handler):
                w_ap, w_t = slab.next()
                P.dma(w_ap, WS[wname][sidx], reads=[ws_tok[wname]], writes=[w_t])
                for j in range(nblk):
                    p_ap, p_t = next_ps()
                    for kc in range(16):
                        mm(p_ap, w_ap[:, kc, j * 128:(j + 1) * 128], xt_ap[:, kc, :], kc == 0, kc == 15, [w_t, xt_t], [p_t])
                    handler(sidx * 4 + j, p_ap, p_t)

            def tm_group(w_ap, w_t, ncols, handler):
                for t4 in range(4):
                    p_ap, p_t = next_ps()
                    for kc in range(16):
                        mm(p_ap[:, 0:ncols], xt_ap[:, kc, t4 * 128:(t4 + 1) * 128], w_ap[:, kc, 0:ncols], kc == 0, kc == 15, [w_t, xt_t], [p_t])
                    handler(t4, p_ap, p_t)

            def ev_store(p_ap, p_t, ncols, dst, dst_tok, func=AF.Copy, scale=1.0):
                o_ap, o_t = oev.next()
                evi[0] += 1
                if func == AF.Copy and evi[0] % 2 == 0 and scale == 1.0:
                    cp("dve", o_ap[:, 0:ncols], p_ap[:, 0:ncols], [p_t], [o_t])
                else:
                    act(o_ap[:, 0:ncols], p_ap[:, 0:ncols], func, [p_t], [o_t], scale=scale)
                P.dma(dst, o_ap[:, 0:ncols], reads=[o_t], writes=[dst_tok], eng="pool")

            def qknorm(p_ap, p_t, wcol, dst, dst_tok):
                sq_ap, sq_t = sqt.next()
                act(sq_ap, p_ap, AF.Square, [p_t], [sq_t])
                q_ap, q_t = psb(6)
                mm(q_ap, BD64, sq_ap, True, True, [sq_t, cb_t], [q_t])
                b_ap, b_t = r1.next()
                rsqrt_act(b_ap, q_ap, [q_t], [b_t], eps=True)
                o_ap, o_t = oev.next()
                stt(o_ap, p_ap, wq8[:, wcol:wcol + 1], b_ap, ALU.mult, ALU.mult, [p_t, b_t, wq8_t], [o_t])
                P.dma(dst, o_ap, reads=[o_t], writes=[dst_tok], eng="pool")

            tsl = slice(ost * 512, (ost + 1) * 512)
            for s in range(6):
                w_ap, w_t = slab.next()
                P.dma(w_ap, WS["kv"][s], reads=[ws_tok["kv"]], writes=[w_t])
                if s < 2:
                    tm_group(w_ap, w_t, 512, lambda t4, p, pt, s=s: ev_store(
                        p, pt, 512, MK[(st * 4 + t4) * 128:(st * 4 + t4 + 1) * 128, s * 512:(s + 1) * 512], scr_tok["mk"]))
                else:
                    tm_group(w_ap, w_t, 512, lambda t4, p, pt, s=s: ev_store(
                        p, pt, 512, MV[(st * 4 + t4) * 128:(st * 4 + t4 + 1) * 128, (s - 2) * 512:(s - 1) * 512], scr_tok["mv"]))
            w_ap, w_t = slab.next()
            P.dma(w_ap[:, :, 0:16], WS_g, reads=[ws_tok["g"]], writes=[w_t])
            P.dma(w_ap[:, :, 256:512], WS_sv, reads=[ws_tok["sv"]], writes=[w_t])
            tm_group(w_ap, w_t, 16, lambda t4, p, pt: cp("dve", GP[:, st * 4 + t4, :], p[:, 0:16], [pt], [GP_t]))
            if own or halo:
                base = (ost + 1) * 512

                def sv_h(t4, p, pt):
                    o_ap, o_t = oev.next()
                    cp("act", o_ap[:, 0:256], p[:, 0:256], [pt], [o_t])
                    P.dma(SV[base + t4 * 128:base + (t4 + 1) * 128, :], o_ap[:, 0:256], reads=[o_t], writes=[scr_tok["sv"]], eng="pool")
                for t4 in range(4):
                    p_ap, p_t = next_ps()
                    for kc in range(16):
                        mm(p_ap[:, 0:256], xt_ap[:, kc, t4 * 128:(t4 + 1) * 128], w_ap[:, kc, 256:512], kc == 0, kc == 15, [w_t, xt_t], [p_t])
                    sv_h(t4, p_ap, p_t)
                for s in range(2):
                    fm_group("sk", s, 4, lambda blk, p, pt: qknorm(p, pt, 1, SK[blk, :, base:base + 512], scr_tok["sk"]))
            if own:
                for s in range(4):
                    w_ap, w_t = slab.next()
                    P.dma(w_ap, WS["o"][s], reads=[ws_tok["o"]], writes=[w_t])
                    tm_group(w_ap, w_t, 512, lambda t4, p, pt, s=s: ev_store(
                        p, pt, 512, MO[(ost * 4 + t4) * 128:(ost * 4 + t4 + 1) * 128, s * 512:(s + 1) * 512], scr_tok["mo"],
                        func=AF.Tanh, scale=0.5))
                for s in range(4):
                    if s < 2:
                        fm_group("qk", s, 4, lambda blk, p, pt: ev_store(p, pt, 512, MQT[blk, :, tsl], scr_tok["mqt"], scale=128 ** -0.5))
                    else:
                        fm_group("qk", s, 4, lambda blk, p, pt: ev_store(p, pt, 512, MKT[blk - 8, :, tsl], scr_tok["mkt"]))
                for s in range(4):
                    fm_group("sq", s, 4, lambda blk, p, pt: qknorm(p, pt, 0, SQ[blk, :, tsl], scr_tok["sq"]))
                for s in range(4):
                    fm_group("ga", s, 4, lambda blk, p, pt: ev_store(p, pt, 512, TGA[blk, :, tsl], scr_tok["tga"], func=AF.Tanh, scale=0.5))
                for s in range(4):
                    fm_group("gb", s, 4, lambda blk, p, pt: ev_store(p, pt, 512, TGB[blk, :, tsl], scr_tok["tgb"], func=AF.Tanh, scale=0.5))
        P.barrier()
        A.reset()
        if dbg:
            for nm in ("mk", "mv", "mo", "mqt", "mkt", "sq", "sk", "sv", "tga"):
                dst, src = dbgs[nm]
                if len(src.shape) == 3:
                    for i in range(src.shape[0]):
                        P.dma(dst[i], src[i], writes=[T()])
                else:
                    n0 = src.shape[0]
                    for i in range(0, n0, 512):
                        P.dma(dst[i:min(n0, i + 512)], src[i:min(n0, i + 512)], writes=[T()])
            P.dma(dbgs["gp"], GP.rearrange("p a b -> p (a b)"), reads=[GP_t], writes=[T()])
            P.barrier()

        gbb, gbb_t = A.tile([128, 16], F32)
        P.dma(gbb[:, 0:8], ml_ib.partition_broadcast(128), writes=[gbb_t])
        P.dma(gbb[:, 8:16], ml_fb.partition_broadcast(128), writes=[gbb_t])
        LI, LI_t = A.tile([128, NTILES, 8], F32)
        LF, LF_t = A.tile([128, NTILES, 8], F32)
        gz, gz_t = A.tile([128, NTILES, 16], F32)
        tt("dve", gz, GP, gbb.unsqueeze(1).to_broadcast([128, NTILES, 16]), ALU.add, [GP_t, gbb_t], [gz_t])
        act(gz, gz, AF.Tanh, [gz_t], [gz_t], scale=1.0 / 15.0)
        ts("dve", LI, gz[:, :, 0:8], 15.0, None, ALU.mult, None, [gz_t], [LI_t])
        act(gz[:, :, 8:16], gz[:, :, 8:16], AF.Exp, [gz_t], [gz_t], scale=-15.0)
        ts("dve", gz[:, :, 8:16], gz[:, :, 8:16], 1.0, None, ALU.add, None, [gz_t], [gz_t])
        act(gz[:, :, 8:16], gz[:, :, 8:16], AF.Ln, [gz_t], [gz_t])
        ts("dve", LF, gz[:, :, 8:16], -1.0, None, ALU.mult, None, [gz_t], [LF_t])
        nwb, nwb_t = A.tile([128, D], F32)
        P.dma(nwb, ml_nw.partition_broadcast(128), writes=[nwb_t])
        ts("dve", nwb, nwb, 0.5, None, ALU.mult, None, [nwb_t], [nwb_t])
        Cst, C_t = A.tile([128, 8, 257], F32)
        Cb, Cb_t = A.tile([128, 8, 257], BF16)
        memset("pool", Cst, 0.0, [C_t])
        memset("pool", Cb, 0.0, [Cb_t])
        vaug = A.ring(2, [128, 8, 257], BF16)
        for va, vt in vaug.items:
            memset("pool", va[:, :, 256:257], 1.0, [vt])
        ktk = A.ring(2, [128, 1024], BF16)
        qTs = A.ring(2, [128, 8, 512], BF16)
        kTs = A.ring(2, [128, 8, 512], BF16)
        mo_r = A.ring(2, [128, D], BF16)
        Gt = A.ring(2, [128, D], F32)
        lfb = A.ring(2, [128, 8, 128], F32)
        smm = A.ring(3, [128, 8, 8], F32)
        dT = A.ring(2, [128, 128], BF16)
        sTs = A.ring(2, [128, 128], BF16)
        t1r = A.ring(2, [128, 257], F32)
        NUM = A.ring(2, [128, 8, 257], F32)
        kw = A.ring(2, [128, 128], BF16)
        mlo = A.ring(2, [128, D], BF16)
        mlts = A.ring(2, [128, 16, 512], BF16)
        mlt_tok = T()
        pending_tail = [None]
        for tix in range(NTILES):
            own = tix >= NPRE // 128
            otix = tix - NPRE // 128
            k_ap, k_t = ktk.next()
            P.dma(k_ap, MK[tix * 128:(tix + 1) * 128, :], reads=[scr_tok["mk"]], writes=[k_t])
            v_ap, v_t = vaug.next()
            P.dma(v_ap[:, :, 0:256], MV[tix * 128:(tix + 1) * 128, :].rearrange("p (h c) -> p h c", c=256),
                  reads=[scr_tok["mv"]], writes=[v_t])
            if own and otix % 4 == 0:
                q_ap, q_t = qTs.next()
                kT_ap, kT_t = kTs.next()
                sl = slice(otix * 128, otix * 128 + 512)
                P.dma(q_ap, MQT[:, :, sl].rearrange("h p c -> p h c"), reads=[scr_tok["mqt"]], writes=[q_t])
                P.dma(kT_ap, MKT[:, :, sl].rearrange("h p c -> p h c"), reads=[scr_tok["mkt"]], writes=[kT_t])
                ms_ap, ms_t = mlts.next()
            bg, bg_t = psb(0)
            mm(bg[:, 0:8], TRI, LF[:, tix, :], True, True, [cf_t, LF_t], [bg_t])
            mm(bg[:, 8:16], ONESF, LF[:, tix, :], True, True, [cf_t, LF_t], [bg_t])
            s_ap, s_t = smm.next()
            tt("dve", s_ap[:, 0, :], LI[:, tix, :], bg[:, 0:8], ALU.subtract, [LI_t, bg_t], [s_t])
            tt("dve", s_ap[:, 1, :], s_ap[:, 0, :], bg[:, 8:16], ALU.add, [s_t, bg_t], [s_t])
            act(s_ap[:, 2, :], s_ap[:, 1, :], AF.Exp, [s_t], [s_t])
            act(s_ap[:, 3, :], bg[:, 0:8], AF.Exp, [bg_t], [s_t])
            act(s_ap[:, 4, :], bg[:, 8:16], AF.Exp, [bg_t], [s_t])
            c0 = (otix % 4) * 128
            if own:
                mo_ap, mo_t = mo_r.next()
                P.dma(mo_ap, MO[otix * 128:(otix + 1) * 128, :], reads=[scr_tok["mo"]], writes=[mo_t])
                g_ap, g_t = Gt.next()
                stt(g_ap, mo_ap, 1.0, nwb, ALU.add, ALU.mult, [mo_t, nwb_t], [g_t])
                lb_ap, lb_t = lfb.next()
                cp("pool", lb_ap, LF[:, tix, :].unsqueeze(2).to_broadcast([128, 8, 128]), [LF_t], [lb_t])
                n_ap, n_t = NUM.next()
                ss_ap = s_ap[:, 5, :]
            sts = {}

            def p1(h, s_ap=s_ap, s_t=s_t):
                bb, bb_t = psb(1)
                mm(bb[:, 0:128], lb_ap[:, h, :], TRI, True, False, [lb_t, cf_t], [bb_t])
                mm(bb[:, 0:128], IDB, NEGM, False, True, [cb_t], [bb_t])
                d_ap, d_t = dT.next()
                act(d_ap, bb[:, 0:128], AF.Exp, [bb_t, s_t], [d_t], bias=s_ap[:, 0, h:h + 1])
                sp_, sp_t = psb(2)
                mm(sp_[:, 0:128], kT_ap[:, h, c0:c0 + 128], q_ap[:, h, c0:c0 + 128], True, True, [kT_t, q_t], [sp_t])
                st_ap, st_t = sTs.next()
                tt("dve", st_ap, sp_[:, 0:128], d_ap, ALU.mult, [sp_t, d_t], [st_t])
                sts[h] = (st_ap, st_t)

            def p2(h, s_ap=s_ap, s_t=s_t):
                if own:
                    st_ap, st_t = sts[h]
                    pi, pi_t = psb(3)
                    mm(pi[:, 0:257], st_ap, v_ap[:, h, :], True, True, [st_t, v_t], [pi_t])
                    pe_, pe_t = psb(4)
                    mm(pe_[:, 0:257], q_ap[:, h, c0:c0 + 128], Cb[:, h, :], True, True, [q_t, Cb_t], [pe_t])
                    t1, t1_t = t1r.next()
                    act(t1, pe_[:, 0:257], AF.Copy, [pe_t, s_t], [t1_t], scale=s_ap[:, 3, h:h + 1])
                    tt("dve", n_ap[:, h, :], t1, pi[:, 0:257], ALU.add, [t1_t, pi_t], [n_t])
                    act(junk[:, 0:256], n_ap[:, h, 0:256], AF.Square, [n_t], [junk_t, s_t], accum=ss_ap[:, h:h + 1])
                kw_ap, kw_t = kw.next()
                act(kw_ap, k_ap[:, h * 128:(h + 1) * 128], AF.Copy, [k_t, s_t], [kw_t], scale=s_ap[:, 2, h:h + 1])
                pc, pc_t = psb(5)
                mm(pc[:, 0:257], kw_ap, v_ap[:, h, :], True, True, [kw_t, v_t], [pc_t])
                stt(Cst[:, h, :], Cst[:, h, :], s_ap[:, 4, h:h + 1], pc[:, 0:257], ALU.mult, ALU.add, [C_t, s_t, pc_t], [C_t])
                cp("act", Cb[:, h, :], Cst[:, h, :], [C_t], [Cb_t])

            def tail(s_ap=s_ap, s_t=s_t, n_ap=n_ap if own else None, n_t=n_t if own else None, ss_ap=ss_ap if own else None,
                     g_ap=g_ap if own else None, g_t=g_t if own else None, ms_ap=ms_ap if own else None, ms_t=ms_t if own else None,
                     c0=c0, otix=otix):
                den = n_ap[:, :, 256]
                tt("dve", s_ap[:, 7, :], den, den, ALU.mult, [n_t], [s_t])
                ts("dve", s_ap[:, 7, :], s_ap[:, 7, :], 1.0, None, ALU.max, None, [s_t], [s_t])
                rsqrt_act(s_ap[:, 6, :], s_ap[:, 7, :], [s_t], [s_t])
                tt("dve", s_ap[:, 7, :], s_ap[:, 6, :], s_ap[:, 6, :], ALU.mult, [s_t], [s_t])
                tt("dve", s_ap[:, 7, :], s_ap[:, 7, :], ss_ap, ALU.mult, [s_t], [s_t])
                ts("dve", s_ap[:, 7, :], s_ap[:, 7, :], 1.0 / 256, EPS, ALU.mult, ALU.add, [s_t], [s_t])
                rsqrt_act(s_ap[:, 1, :], s_ap[:, 7, :], [s_t], [s_t])
                tt("dve", s_ap[:, 1, :], s_ap[:, 1, :], s_ap[:, 6, :], ALU.mult, [s_t], [s_t])
                o_ap, o_t = mlo.next()
                for h in range(8):
                    stt(o_ap[:, h * 256:(h + 1) * 256], n_ap[:, h, 0:256], s_ap[:, 1, h:h + 1], g_ap[:, h * 256:(h + 1) * 256],
                        ALU.mult, ALU.mult, [n_t, s_t, g_t], [o_t])
                for half in range(2):
                    pb, pb_t = psb(6 + half, BF16)
                    for k in range(8):
                        kc = half * 8 + k
                        tr(pb[:, k * 128:(k + 1) * 128], o_ap[:, kc * 128:(kc + 1) * 128], IDB, [o_t, cb_t], [pb_t])
                    cp("act", ms_ap[:, half * 8:(half + 1) * 8, c0:c0 + 128], pb[:, 0:1024].rearrange("p (k c) -> p k c", c=128), [pb_t], [ms_t])
                if otix % 4 == 3:
                    sl = slice((otix - 3) * 128, (otix + 1) * 128)
                    P.dma(MLT[:, :, sl].rearrange("k p c -> p k c"), ms_ap, reads=[ms_t], writes=[mlt_tok], eng="pool")

            if own:
                p1(0)
            for h in range(8):
                if own and h + 1 < 8:
                    p1(h + 1)
                p2(h)
                if h == 2 and pending_tail[0] is not None:
                    pending_tail[0]()
                    pending_tail[0] = None
            if own:
                pending_tail[0] = tail
        if pending_tail[0] is not None:
            pending_tail[0]()
        P.barrier()
        A.reset()

        ebm, ebm_t = A.tile([128, 4, 2, 2, 512], BF16)
        P.dma(ebm, ebm_in.rearrange("p (g k a c) -> p g k a c", g=4, k=2, a=2), writes=[ebm_t])
        eb0, eb0_t = A.tile([128, 4, 2, 512], BF16)
        ts("dve", eb0, ebm[:, :, 0, :, :], flag[:, 0:1], None, ALU.mult, None, [ebm_t, flag_t], [eb0_t])
        sinkN, sinkE_t = A.tile([128, 32], F32)
        P.dma(sinkN, sw_sinks.partition_broadcast(128), writes=[sinkE_t])
        act(sinkN, sinkN, AF.Exp, [sinkE_t], [sinkE_t])
        sinkE = sinkN.rearrange("p (g j a) -> p g a j", g=4, a=2)
        sqs = A.ring(2, [128, 16, 512], BF16)
        sks = A.ring(2, [128, 8, 640], BF16)
        vau = A.ring(3, [128, 4, 65], BF16)
        for va, vt in vau.items:
            memset("pool", va[:, :, 64:65], 1.0, [vt])
        eT = A.ring(8, [128, 512], BF16)
        eM = A.ring(8, [128, 512], BF16)
        rden = A.ring(2, [128, 2, 4], F32)
        swo = A.ring(2, [128, 512], BF16)
        swts = A.ring(2, [128, 16, 512], BF16)
        swt_tok = T()
        vprev = None
        pending_pv = [None]
        for n in range(-1, 32):
            va, vt = vau.next()
            r0_ = 512 + n * 128
            P.dma(va[:, :, 0:64], SV[r0_:r0_ + 128, :].rearrange("p (g c) -> p g c", c=64), reads=[scr_tok["sv"]], writes=[vt])
            if n < 0:
                vprev = (va, vt)
                continue
            if n % 4 == 0:
                q_ap, q_t = sqs.next()
                P.dma(q_ap, SQ[:, :, n * 128:n * 128 + 512].rearrange("k p c -> p k c"), reads=[scr_tok["sq"]], writes=[q_t])
                k_ap, k_t = sks.next()
                P.dma(k_ap, SK[:, :, 384 + n * 128:384 + n * 128 + 640].rearrange("k p c -> p k c"), reads=[scr_tok["sk"]], writes=[k_t])
                ws_ap, ws_t = swts.next()
            c0 = (n % 4) * 128
            for g in range(4):
                em = {}
                for kb in range(2):
                    kc0 = c0 + kb * 128
                    for ab in range(2):
                        p_ap, p_t = psb(kb * 2 + ab)
                        mm(p_ap.rearrange("p (j c) -> p j c", c=128), k_ap[:, g * 2 + ab, kc0:kc0 + 128], q_ap[:, 4 * g:4 * g + 4, c0:c0 + 128], True, True, [k_t, q_t], [p_t])
                        e_ap, e_t = eT.next()
                        act(e_ap, p_ap, AF.Exp, [p_t], [e_t])
                        m_ap, m_t = eM.next()
                        if n == 0 and kb == 0:
                            msk, msk_t = eb0[:, g, ab, :], eb0_t
                        else:
                            msk, msk_t = ebm[:, g, kb, ab, :], ebm_t
                        tt("pool" if ab else "dve", m_ap, e_ap, msk, ALU.mult, [e_t, msk_t], [m_t])
                        em[(kb, ab)] = (m_ap, m_t)

                def pv_phase(em=em, g=g, n=n, c0=c0, vprev=vprev, va=va, vt=vt, ws_ap=ws_ap, ws_t=ws_t):
                    o_ap, o_t = swo.next()
                    rd_ap, rd_t = rden.next()
                    for ab in range(2):
                        po, po_t = psb(4 + ab)
                        pov = po[:, 0:260].rearrange("p (j c) -> p j c", c=65)
                        for j in range(4):
                            for kb in range(2):
                                m_ap, m_t = em[(kb, ab)]
                                vv, vvt = (vprev if kb == 0 else (va, vt))
                                mm(pov[:, j, :], m_ap[:, j * 128:(j + 1) * 128], vv[:, g, :], kb == 0, kb == 1, [m_t, vvt], [po_t])
                        tt("dve", rd_ap[:, ab, :], pov[:, :, 64], sinkE[:, g, ab, :], ALU.add, [po_t, sinkE_t], [rd_t])
                        P.op("dve", lambda e, rd_ap=rd_ap, ab=ab: e.reciprocal(out=rd_ap[:, ab, :], in_=rd_ap[:, ab, :]), [rd_t], [rd_t])
                        ov = o_ap.rearrange("p (j a c) -> p j a c", a=2, c=64)
                        tt("dve", ov[:, :, ab, :], pov[:, :, 0:64], rd_ap[:, ab, :].unsqueeze(2).to_broadcast([128, 4, 64]), ALU.mult,
                           [po_t, rd_t], [o_t])
                    pb, pb_t = psb(6 + g % 2, BF16)
                    for j in range(4):
                        tr(pb[:, j * 128:(j + 1) * 128], o_ap[:, j * 128:(j + 1) * 128], IDB, [o_t, cb_t], [pb_t])
                    cp("act", ws_ap[:, 4 * g:4 * g + 4, c0:c0 + 128], pb[:, 0:512].rearrange("p (k c) -> p k c", c=128), [pb_t], [ws_t])
                    if n % 4 == 3 and g == 3:
                        sl = slice((n - 3) * 128, (n + 1) * 128)
                        P.dma(SWT[:, :, sl].rearrange("k p c -> p k c"), ws_ap, reads=[ws_t], writes=[swt_tok], eng="pool")

                if pending_pv[0] is not None:
                    pending_pv[0]()
                pending_pv[0] = pv_phase
            vprev = (va, vt)
        if pending_pv[0] is not None:
            pending_pv[0]()
        if dbg:
            for i in range(16):
                P.dma(dbgs["mlt"][i], MLT[i], reads=[mlt_tok], writes=[T()])
                P.dma(dbgs["swt"][i], SWT[i], reads=[swt_tok], writes=[T()])
        P.barrier()
        A.reset()

        mls = A.ring(2, [128, 16, 512], BF16)
        sws = A.ring(2, [128, 16, 512], BF16)
        was = A.ring(2, [128, 16, 512], BF16)
        wbs = A.ring(2, [128, 16, 512], BF16)
        tg = A.ring(4, [128, 512], BF16)
        uv = A.ring(4, [128, 512], F32)
        mxo = A.ring(3, [128, 512], BF16)
        mixt_tok = T()
        for st in range(8):
            tsl = slice(st * 512, (st + 1) * 512)
            ml_ap, ml_t = mls.next()
            sw_ap, sw_t = sws.next()
            P.dma(ml_ap, MLT[:, :, tsl].rearrange("k p c -> p k c"), reads=[mlt_tok], writes=[ml_t])
            P.dma(sw_ap, SWT[:, :, tsl].rearrange("k p c -> p k c"), reads=[swt_tok], writes=[sw_t])
            for s in range(4):
                wa_ap, wa_t = was.next()
                wb_ap, wb_t = wbs.next()
                P.dma(wa_ap, WS["a"][s], reads=[ws_tok["a"]], writes=[wa_t])
                P.dma(wb_ap, WS["b"][s], reads=[ws_tok["b"]], writes=[wb_t])
                for j in range(4):
                    ch = 4 * s + j
                    ta_ap, ta_t = tg.next()
                    tb_ap, tb_t = tg.next()
                    P.dma(ta_ap, TGA[ch, :, tsl], reads=[scr_tok["tga"]], writes=[ta_t])
                    P.dma(tb_ap, TGB[ch, :, tsl], reads=[scr_tok["tgb"]], writes=[tb_t])
                    pa, pa_t = psb((ch % 2) * 2)
                    pbk, pbk_t = psb((ch % 2) * 2 + 1)
                    for kc in range(16):
                        mm(pa, wa_ap[:, kc, j * 128:(j + 1) * 128], ml_ap[:, kc, :], kc == 0, kc == 15, [wa_t, ml_t], [pa_t])
                    for kc in range(16):
                        mm(pbk, wb_ap[:, kc, j * 128:(j + 1) * 128], sw_ap[:, kc, :], kc == 0, kc == 15, [wb_t, sw_t], [pbk_t])
                    u_ap, u_t = uv.next()
                    v_ap, v_t = uv.next()
                    stt(u_ap, ta_ap, 1.0, pa, ALU.add, ALU.mult, [ta_t, pa_t], [u_t])
                    stt(v_ap, tb_ap, 1.0, pbk, ALU.add, ALU.mult, [tb_t, pbk_t], [v_t])
                    m_ap, m_t = mxo.next()
                    tt("pool", m_ap, u_ap, v_ap, ALU.add, [u_t, v_t], [m_t])
                    P.dma(MIXT[ch, :, tsl], m_ap, reads=[m_t], writes=[mixt_tok], eng="pool")
        P.barrier()
        A.reset()
        if dbg:
            for i in range(16):
                P.dma(dbgs["mixt"][0][i], MIXT[i], writes=[T()])
            P.barrier()

        wo, wo_t = A.tile([128, 4, 16, 512], BF16)
        for s in range(4):
            P.dma(wo[:, s], WS["out"][s], reads=[ws_tok["out"]], writes=[wo_t])
        wr, wr_t = A.tile([128, 16, 32], F32)
        P.dma(wr, w_router.rearrange("(k p) e -> p k e", p=128), writes=[wr_t])
        tt("dve", wr, wr, rs[:, 1, :].unsqueeze(2).to_broadcast([128, 16, 32]), ALU.mult, [wr_t, rs_t], [wr_t])
        brb, brb_t = A.tile([128, 32], F32)
        P.dma(brb, b_router.partition_broadcast(128), writes=[brb_t])
        eoff, eoff_t = A.tile([128, 32], F32)
        P.op("pool", lambda e: e.iota(eoff, pattern=[[CAP, 32]], base=0, channel_multiplier=0, allow_small_or_imprecise_dtypes=True), (), [eoff_t])
        cntB, cntB_t = A.tile([128, 32], F32)
        memset("pool", cntB, 0.0, [cntB_t])
        mxs = A.ring(2, [128, 16, 512], BF16)
        xin = A.ring(1, [128, D], F32)
        h1r = A.ring(2, [128, D], F32)
        xnf = A.ring(1, [128, D], F32)
        xnb = A.ring(2, [128, D], BF16)
        xTf = A.ring(1, [128, 16, 128], F32)
        sm = A.ring(4, [128, 4], F32)
        rt = A.ring(2, [128, 16, 32], F32)
        t8 = A.ring(2, [128, 8], F32)
        h1_tok, xg_tok = T(), T()
        for tix in range(32):
            if tix % 4 == 0:
                mx_ap, mx_t = mxs.next()
                P.dma(mx_ap, MIXT[:, :, tix * 128:tix * 128 + 512].rearrange("k p c -> p k c"), reads=[mixt_tok], writes=[mx_t])
            c0 = (tix % 4) * 128
            x_ap, x_t = xin.next()
            P.dma(x_ap, xall[NPRE + tix * 128:NPRE + (tix + 1) * 128, :], writes=[x_t])
            h_ap, h_t = h1r.next()
            for s in range(4):
                p_ap, p_t = psb(s % 2)
                for kc in range(16):
                    mm(p_ap, mx_ap[:, kc, c0:c0 + 128], wo[:, s, kc, :], kc == 0, kc == 15, [mx_t, wo_t], [p_t])
                tt("dve", h_ap[:, s * 512:(s + 1) * 512], p_ap, x_ap[:, s * 512:(s + 1) * 512], ALU.add, [p_t, x_t], [h_t])
            P.dma(H1[tix * 128:(tix + 1) * 128, :], h_ap, reads=[h_t], writes=[h1_tok], eng="pool")
            if dbg:
                P.dma(dbgs["h1"][tix * 128:(tix + 1) * 128, :], h_ap, reads=[h_t], writes=[T()])
            s_ap, s_t = sm.next()
            act(junk, h_ap, AF.Square, [h_t], [junk_t, s_t], accum=s_ap[:, 0:1])
            rsqrt_act(s_ap[:, 2:3], s_ap[:, 0:1], [s_t], [s_t], scale=1.0 / D, eps=True)
            xb_ap, xb_t = xnb.next()
            act(xb_ap, h_ap, AF.Copy, [h_t, s_t], [xb_t], scale=s_ap[:, 2:3])
            xf_ap, xf_t = xnf.next()
            act(xf_ap, h_ap, AF.Copy, [h_t, s_t], [xf_t], scale=s_ap[:, 2:3])
            xT_ap, xT_t = xTf.next()
            for q4 in range(4):
                pt_, pt_t = psb(2 + q4 % 2)
                for k in range(4):
                    kc = q4 * 4 + k
                    tr(pt_[:, k * 128:(k + 1) * 128], xf_ap[:, kc * 128:(kc + 1) * 128], IDF, [xf_t, cf_t], [pt_t])
                cp("act", xT_ap[:, q4 * 4:(q4 + 1) * 4, :], pt_.rearrange("p (k c) -> p k c", c=128), [pt_t], [xT_t])
            pl, pl_t = psb(4)
            for kc in range(16):
                mm(pl[:, 0:32], xT_ap[:, kc, :], wr[:, kc, :], kc == 0, kc == 15, [xT_t, wr_t], [pl_t])
            r_ap, r_t = rt.next()
            lg = r_ap[:, 0, :]
            tt("dve", lg, pl[:, 0:32], brb, ALU.add, [pl_t, brb_t], [r_t])
            t_ap, t_t = t8.next()
            P.op("dve", lambda e, t_ap=t_ap, lg=lg: e.max(out=t_ap, in_=lg), [r_t], [t_t])
            ts("dve", t_ap[:, 4:8], t_ap[:, 0:4], t_ap[:, 0:1], None, ALU.subtract, None, [t_t], [t_t])
            act(t_ap[:, 4:8], t_ap[:, 4:8], AF.Exp, [t_t], [t_t])
            P.op("dve", lambda e, t_ap=t_ap, s_ap=s_ap: e.reduce_sum(out=s_ap[:, 3:4], in_=t_ap[:, 4:8], axis=AX.X), [t_t], [s_t])
            P.op("dve", lambda e, s_ap=s_ap: e.reciprocal(out=s_ap[:, 3:4], in_=s_ap[:, 3:4]), [s_t], [s_t])
            ts("dve", t_ap[:, 4:8], t_ap[:, 4:8], s_ap[:, 3:4], None, ALU.mult, None, [t_t, s_t], [t_t])
            msk = r_ap[:, 1, :]
            ts("dve", msk, lg, t_ap[:, 3:4], None, ALU.is_ge, None, [r_t, t_t], [r_t])
            mb = r_ap[:, 2, :].bitcast(BF16)[:, 0:32]
            cp("dve", mb, msk, [r_t], [r_t])
            pp, pp_t = psb(5)
            mm(pp[:, 0:32], TRIS, mb, True, True, [cb_t, r_t], [pp_t])
            pos = r_ap[:, 3, :]
            tt("dve", pos, pp[:, 0:32], cntB, ALU.add, [pp_t, cntB_t], [r_t])
            pc2, pc2_t = psb(7)
            mm(pc2[:, 0:32], ONESF, msk, True, True, [cf_t, r_t], [pc2_t])
            tt("dve", cntB, cntB, pc2[:, 0:32], ALU.add, [pc2_t, cntB_t], [cntB_t])
            for k in range(4):
                oh = r_ap[:, 4 + k, :]
                ts("dve", oh, lg, t_ap[:, k:k + 1], None, ALU.is_equal, None, [r_t, t_t], [r_t])
                pk = r_ap[:, 8 + k, :]
                tt("dve", pk, oh, pos, ALU.mult, [r_t], [r_t])
                P.op("dve", lambda e, pk=pk, r_ap=r_ap, k=k: e.reduce_sum(out=r_ap[:, 12, k:k + 1], in_=pk, axis=AX.X), [r_t], [r_t])
                tt("dve", pk, oh, eoff, ALU.mult, [r_t, eoff_t], [r_t])
                P.op("dve", lambda e, pk=pk, r_ap=r_ap, k=k: e.reduce_sum(out=r_ap[:, 12, 4 + k:5 + k], in_=pk, axis=AX.X), [r_t], [r_t])
            okk = r_ap[:, 12, 8:12]
            ts("dve", okk, r_ap[:, 12, 0:4], float(CAP), None, ALU.is_lt, None, [r_t], [r_t])
            slf = r_ap[:, 12, 12:16]
            tt("dve", slf, r_ap[:, 12, 0:4], r_ap[:, 12, 4:8], ALU.add, [r_t], [r_t])
            ts("dve", slf, slf, float(TRASH), None, ALU.subtract, None, [r_t], [r_t])
            tt("dve", slf, slf, okk, ALU.mult, [r_t], [r_t])
            ts("dve", slf, slf, float(TRASH), None, ALU.add, None, [r_t], [r_t])
            cp("dve", SLOT[:, tix, :], slf, [r_t], [SLOT_t])
            tt("dve", GATE[:, tix, :], t_ap[:, 4:8], okk, ALU.mult, [t_t, r_t], [GATE_t])
            for k in range(4):
                P.op("pool", lambda e, k=k, tix=tix, xb_ap=xb_ap: e.indirect_dma_start(
                    out=XG, out_offset=bass.IndirectOffsetOnAxis(ap=SLOT[:, tix, k:k + 1], axis=0), in_=xb_ap, in_offset=None),
                    [xb_t, SLOT_t], [xg_tok], dma=True)
        P.barrier()
        A.reset()

        stg = A.ring(2, [128, 16, 512], F32)
        wbf = A.ring(2, [128, 16, 512], BF16)
        xgl = A.ring(4, [128, D], BF16)
        xgT, xgT_t = A.tile([128, 16, CAP], BF16)
        actT, actT_t = A.tile([128, 16, CAP], BF16)
        bupr = A.ring(2, [128, 32], F32)
        bdb = A.ring(2, [1, D], BF16)
        ones1, ones1_t = A.tile([1, 128], BF16)
        memset("pool", ones1, 1.0, [ones1_t])
        gl = A.ring(2, [128, CAP // 2], F32)
        sg = A.ring(2, [128, CAP // 2], F32)
        l1 = A.ring(2, [128, CAP // 2], F32)
        yo = A.ring(3, [128, 512], F32)
        ys_tok = T()
        zrow, zrow_t = A.tile([128, 512], F32)
        memset("pool", zrow, 0.0, [zrow_t])
        for s in range(4):
            P.dma(YS[NSLOT:NSLOT + 128, s * 512:(s + 1) * 512], zrow, reads=[zrow_t], writes=[ys_tok])
        NH = CAP // 2
        estate = {}

        def prologue(e_):
            for r0, nr in ETILES:
                g_ap, g_t = xgl.next()
                P.dma(g_ap[0:nr, :], XG[e_ * CAP + r0:e_ * CAP + r0 + nr, :], reads=[xg_tok], writes=[g_t])
                for half in range(2):
                    pb, pb_t = psb(6 + half, BF16)
                    for k in range(8):
                        kc = half * 8 + k
                        tr(pb[:, k * 128:k * 128 + nr], g_ap[0:nr, kc * 128:(kc + 1) * 128], IDB[0:nr, 0:nr], [g_t, cb_t], [pb_t])
                    cp("act" if half else "dve", xgT[:, half * 8:(half + 1) * 8, r0:r0 + nr],
                       pb[:, 0:1024].rearrange("p (k c) -> p k c", c=128)[:, :, 0:nr], [pb_t], [xgT_t])
            bup, bup_t = bupr.next()
            P.dma(bup, b_up[e_].rearrange("(c p) -> p c", p=128), writes=[bup_t], allow_slow_non_contiguous=True)
            bb_ap, bb_t = bdb.next()
            P.dma(bb_ap, b_dn[e_:e_ + 1, :], writes=[bb_t], eng="pool")
            estate[e_] = (bup, bup_t, bb_ap, bb_t)

        def up_task(e_, jj):
            st_ = {}

            def load():
                s_ap, s_t = stg.next()
                P.dma(s_ap[:, :, 0:256], w_up[e_, :, jj * 256:(jj + 1) * 256].rearrange("(k p) c -> p k c", p=128), writes=[s_t])
                P.dma(s_ap[:, :, 256:512], w_up[e_, :, D + jj * 256:D + (jj + 1) * 256].rearrange("(k p) c -> p k c", p=128), writes=[s_t])
                st_["s"] = (s_ap, s_t)

            def cast():
                s_ap, s_t = st_["s"]
                w_ap, w_t = wbf.next()
                for kc in range(16):
                    cast_scale(w_ap[:, kc, :], s_ap[:, kc, :], rs[:, 1, kc:kc + 1], [s_t, rs_t], [w_t])
                st_["w"] = (w_ap, w_t)

            def compute():
                w_ap, w_t = st_["w"]
                bup, bup_t, _, _ = estate[e_]
                for hf in range(2):
                    ch = jj * 2 + hf
                    for nh in range(2):
                        pg, pg_t = psb(nh * 2)
                        plin, plin_t = psb(nh * 2 + 1)
                        for kc in range(16):
                            mm(pg[:, 0:NH], w_ap[:, kc, hf * 128:(hf + 1) * 128], xgT[:, kc, nh * NH:(nh + 1) * NH], kc == 0, kc == 15, [w_t, xgT_t], [pg_t])
                        for kc in range(16):
                            mm(plin[:, 0:NH], w_ap[:, kc, 256 + hf * 128:256 + (hf + 1) * 128], xgT[:, kc, nh * NH:(nh + 1) * NH], kc == 0, kc == 15, [w_t, xgT_t], [plin_t])
                        g_ap, g_t = gl.next()
                        ts("dve", g_ap, pg[:, 0:NH], bup[:, ch:ch + 1], 7.0, ALU.add, ALU.min, [pg_t, bup_t], [g_t])
                        sg_ap, sg_t = sg.next()
                        act(sg_ap, g_ap, AF.Tanh, [g_t], [sg_t], scale=0.851)
                        l_ap, l_t = l1.next()
                        ts("dve", l_ap, plin[:, 0:NH], bup[:, 16 + ch:17 + ch], 7.0, ALU.add, ALU.min, [plin_t, bup_t], [l_t])
                        ts("dve", l_ap, l_ap, -7.0, 1.0, ALU.max, ALU.add, [l_t], [l_t])
                        stt(sg_ap, sg_ap, 1.0, g_ap, ALU.add, ALU.mult, [sg_t, g_t], [sg_t])
                        tt("dve", actT[:, ch, nh * NH:(nh + 1) * NH], sg_ap, l_ap, ALU.mult, [sg_t, l_t], [actT_t])
            return load, cast, compute

        def down_task(e_, s):
            st_ = {}

            def load():
                s_ap, s_t = stg.next()
                P.dma(s_ap, w_dn[e_, :, s * 512:(s + 1) * 512].rearrange("(k p) c -> p k c", p=128), writes=[s_t])
                st_["s"] = (s_ap, s_t)

            def cast():
                s_ap, s_t = st_["s"]
                w_ap, w_t = wbf.next()
                for kc in range(16):
                    cast_scale(w_ap[:, kc, :], s_ap[:, kc, :], rsh[:, 0:1], [s_t, half_t], [w_t])
                st_["w"] = (w_ap, w_t)

            def compute():
                w_ap, w_t = st_["w"]
                _, _, bb_ap, bb_t = estate[e_]
                for tt_, (r0, nr) in enumerate(ETILES):
                    pd, pd_t = psb(4 + tt_ % 2)
                    for kc in range(16):
                        mm(pd[0:nr, :], actT[:, kc, r0:r0 + nr], w_ap[:, kc, :], kc == 0, False, [actT_t, w_t], [pd_t])
                    mm(pd[0:nr, :], ones1[:, 0:nr], bb_ap[:, s * 512:(s + 1) * 512], False, True, [ones1_t, bb_t], [pd_t])
                    y_ap, y_t = yo.next()
                    cp("act", y_ap[0:nr, :], pd[0:nr, :], [pd_t], [y_t])
                    P.dma(YS[e_ * CAP + r0:e_ * CAP + r0 + nr, s * 512:(s + 1) * 512], y_ap[0:nr, :], reads=[y_t], writes=[ys_tok], eng="pool")
                if s == 0 and e_ + 1 < NE:
                    prologue(e_ + 1)
            return load, cast, compute

        tasks = []
        for e_ in range(NE):
            for jj in range(8):
                tasks.append(up_task(e_, jj))
            for s in range(4):
                tasks.append(down_task(e_, s))
        prologue(0)
        tasks[0][0]()
        tasks[1][0]()
        tasks[0][1]()
        for i in range(len(tasks)):
            if i + 2 < len(tasks):
                tasks[i + 2][0]()
            if i + 1 < len(tasks):
                tasks[i + 1][1]()
            tasks[i][2]()
        P.barrier()
        A.reset()

        wpg, wpg_t = A.tile([128, 4, 16, 512], BF16)
        for s in range(4):
            P.dma(wpg[:, s], WS["pg"][s], reads=[ws_tok["pg"]], writes=[wpg_t])
        wpp, wpp_t = A.tile([128, 2, D], BF16)
        for kc in range(2):
            s_ap, s_t = A.tile([128, D], F32)
            P.dma(s_ap, w_pp[kc * 128:(kc + 1) * 128, :], writes=[s_t])
            cp("act", wpp[:, kc, :], s_ap, [s_t], [wpp_t])
        h2r = A.ring(2, [128, D], F32)
        ysr = A.ring(4, [128, D], F32)
        sm = A.ring(4, [128, 4], F32)
        xnb = A.ring(2, [128, D], BF16)
        xT3 = A.ring(2, [128, 16, 128], BF16)
        pin = A.ring(2, [128, 256], F32)
        pbf = A.ring(2, [128, 256], BF16)
        pT = A.ring(2, [128, 2, 128], BF16)
        tgr = A.ring(2, [128, 512], F32)
        tmr = A.ring(2, [128, 512], F32)
        outr = A.ring(2, [128, D], F32)
        out_tok = T()
        for tix in range(32):
            h_ap, h_t = h2r.next()
            P.dma(h_ap, H1[tix * 128:(tix + 1) * 128, :], reads=[h1_tok], writes=[h_t])
            for k in range(4):
                y_ap, y_t = ysr.next()
                P.op("pool", lambda e, k=k, tix=tix, y_ap=y_ap: e.indirect_dma_start(
                    out=y_ap, out_offset=None, in_=YS, in_offset=bass.IndirectOffsetOnAxis(ap=SLOT[:, tix, k:k + 1], axis=0)),
                    [ys_tok, SLOT_t], [y_t], dma=True)
                stt(h_ap, y_ap, GATE[:, tix, k:k + 1], h_ap, ALU.mult, ALU.add, [y_t, GATE_t, h_t], [h_t])
            if dbg:
                P.dma(dbgs["h2"][tix * 128:(tix + 1) * 128, :], h_ap, reads=[h_t], writes=[T()])
            s_ap, s_t = sm.next()
            act(junk, h_ap, AF.Square, [h_t], [junk_t, s_t], accum=s_ap[:, 0:1])
            rsqrt_act(s_ap[:, 2:3], s_ap[:, 0:1], [s_t], [s_t], scale=1.0 / D, eps=True)
            xb_ap, xb_t = xnb.next()
            act(xb_ap, h_ap, AF.Copy, [h_t, s_t], [xb_t], scale=s_ap[:, 2:3])
            xT_ap, xT_t = xT3.next()
            for half in range(2):
                pb, pb_t = psb(6 + half, BF16)
                for k in range(8):
                    kc = half * 8 + k
                    tr(pb[:, k * 128:(k + 1) * 128], xb_ap[:, kc * 128:(kc + 1) * 128], IDB, [xb_t, cb_t], [pb_t])
                cp("act", xT_ap[:, half * 8:(half + 1) * 8, :], pb[:, 0:1024].rearrange("p (k c) -> p k c", c=128), [pb_t], [xT_t])
            pi_ap, pi_t = pin.next()
            P.dma(pi_ap, p_in[tix * 128:(tix + 1) * 128, :], writes=[pi_t])
            pb_ap, pbt = pbf.next()
            cp("pool", pb_ap, pi_ap, [pi_t], [pbt])
            pT_ap, pT_t = pT.next()
            pq, pq_t = psb(5, BF16)
            for kc in range(2):
                tr(pq[:, kc * 128:(kc + 1) * 128], pb_ap[:, kc * 128:(kc + 1) * 128], IDB, [pbt, cb_t], [pq_t])
            cp("dve", pT_ap, pq[:, 0:256].rearrange("p (k c) -> p k c", c=128), [pq_t], [pT_t])
            o_ap, o_t = outr.next()
            for s in range(4):
                pgt, pgt_t = psb(s % 2)
                for kc in range(16):
                    mm(pgt, xT_ap[:, kc, :], wpg[:, s, kc, :], kc == 0, kc == 15, [xT_t, wpg_t], [pgt_t])
                ppe, ppe_t = psb(2 + s % 2)
                for kc in range(2):
                    mm(ppe, pT_ap[:, kc, :], wpp[:, kc, s * 512:(s + 1) * 512], kc == 0, kc == 1, [pT_t, wpp_t], [ppe_t])
                tg_ap, tg_t = tgr.next()
                act(tg_ap, pgt, AF.Tanh, [pgt_t], [tg_t], scale=0.5)
                tm_ap, tm_t = tmr.next()
                stt(tm_ap, tg_ap, 1.0, ppe, ALU.add, ALU.mult, [tg_t, ppe_t], [tm_t])
                stt(o_ap[:, s * 512:(s + 1) * 512], tm_ap, 0.5, h_ap[:, s * 512:(s + 1) * 512], ALU.mult, ALU.add, [tm_t, h_t], [o_t])
            P.dma(y_out[tix * 128:(tix + 1) * 128, :], o_ap, reads=[o_t], writes=[out_tok], eng="act")
        P.op("sp", lambda e: e.nop(), [out_tok], [])
        P.barrier()

        run = P.emit(sems, dsems)

        @block.sync
        def _(e):
            run("sp", e)

        @block.scalar
        def _(e):
            run("act", e)

        @block.vector
        def _(e):
            run("dve", e)

        @block.gpsimd
        def _(e):
            run("pool", e)

        @block.tensor
        def _(e):
            run("pe", e)
    return nc


def _consts():
    i = np.arange(128)
    ident = np.eye(128, dtype=np.float32)
    tri = (i[:, None] <= i[None, :]).astype(np.float32)
    ones = np.ones((128, 128), np.float32)
    cst_f = np.stack([ident, tri, ones], axis=1)
    negm = np.where(i[None, :] < i[:, None], -30000.0, 0.0).astype(np.float32)
    bd = (i[:, None] // 64 == i[None, :] // 64).astype(np.float32) / 64.0
    tris = (i[:, None] < i[None, :]).astype(np.float32)
    cst_b = np.stack([ident, negm, bd, tris], axis=1).astype(ml_dtypes.bfloat16)
    slopes = 2.0 ** (-8.0 * np.arange(1, 33, dtype=np.float64) / 32)
    eb = np.zeros((128, 4, 2, 2, 4, 128), np.float64)
    q = np.arange(128)
    for g in range(4):
        for kb in range(2):
            kpos = np.arange(128) + 128 * kb
            dist = (q[None, :] + 128) - kpos[:, None]
            win = (dist >= 0) & (dist < 128)
            for ab in range(2):
                for j in range(4):
                    h = 8 * g + 2 * j + ab
                    eb[:, g, kb, ab, j, :] = np.where(win, np.exp(-slopes[h] * dist), 0.0)
    ebm = eb.reshape(128, -1).astype(ml_dtypes.bfloat16)
    return cst_f, cst_b, ebm


_NC_CACHE = {}


def kernel(x, p, norm_mix_w, w_in, ml_igate_b, ml_fgate_b, ml_norm_w, sw_q_norm_w, sw_k_norm_w, sw_sinks,
           w_branch_a, w_branch_b, w_out, norm_ffn_w, w_router, b_router, w_expert_up, b_expert_up,
           w_expert_down, b_expert_down, norm_ple_w, w_ple_gate, w_ple_proj, _dbg=False, _ncores=8):
    f = lambda a: np.ascontiguousarray(np.asarray(a, dtype=np.float32))
    x = f(x)
    p = f(p)[0]
    cst_f, cst_b, ebm = _consts()
    shared = {
        "cst_f": cst_f, "cst_b": cst_b, "ebm": ebm,
        "w_in": f(w_in)[0], "norm_mix_w": f(norm_mix_w)[0], "ml_igate_b": f(ml_igate_b)[0], "ml_fgate_b": f(ml_fgate_b)[0],
        "ml_norm_w": f(ml_norm_w)[0], "sw_q_norm_w": f(sw_q_norm_w)[0], "sw_k_norm_w": f(sw_k_norm_w)[0],
        "sw_sinks": f(sw_sinks)[0], "w_branch_a": f(w_branch_a)[0], "w_branch_b": f(w_branch_b)[0], "w_out": f(w_out)[0],
        "norm_ffn_w": f(norm_ffn_w)[0], "w_router": f(w_router)[0], "b_router": f(b_router)[0],
        "w_expert_up": f(w_expert_up)[0], "b_expert_up": f(b_expert_up)[0], "w_expert_down": f(w_expert_down)[0],
        "b_expert_down": f(b_expert_down)[0], "norm_ple_w": f(norm_ple_w)[0], "w_ple_gate": f(w_ple_gate)[0],
        "w_ple_proj": f(w_ple_proj)[0],
    }
    in_maps = []
    for c in range(_ncores):
        b, hf = c // 2, c % 2
        xall = np.zeros((NT_ALL, D), np.float32)
        if hf == 1:
            xall[:NPRE] = x[b, TOK - NPRE:TOK]
        xall[NPRE:] = x[b, hf * TOK:(hf + 1) * TOK]
        m = dict(shared)
        m["xall"] = xall
        m["p_own"] = np.ascontiguousarray(p[b, hf * TOK:(hf + 1) * TOK])
        m["flag"] = np.full((128, 1), float(hf), np.float32)
        in_maps.append(m)
    key = bool(_dbg)
    if key not in _NC_CACHE:
        _NC_CACHE[key] = build_nc(dbg=_dbg)
    nc = _NC_CACHE[key]
    res = run_bass_kernel_spmd(nc, in_maps, core_ids=list(range(_ncores)))
    out = np.zeros((4, 2 * TOK, D), np.float32)
    for c in range(_ncores):
        out[c // 2, (c % 2) * TOK:(c % 2 + 1) * TOK] = res.results[c]["y"]
    if _dbg:
        return out, res.results
    return out
```
